# Optimizing a Trainium2 kernel written in Bass

```python
import jax, jax.numpy as jnp
from jax import lax
import numpy as np

D_MODEL = 2048
BATCH = 2
SEQ = 8192
DEPTH = 1

GRID_W = 64
NA_HEADS = 8
NA_HEAD_DIM = 128
NA_WIDTH = NA_HEADS * NA_HEAD_DIM
NA_KH = 8
NA_KW = 16
NA_COLS = 3 * NA_WIDTH
RWKV_HEAD_DIM = 64
RWKV_HEADS = 16
RWKV_WIDTH = RWKV_HEADS * RWKV_HEAD_DIM
DECAY_LORA = 96
AAA_LORA = 96
GATE_LORA = 256
RWKV_GN_EPS = 64e-5
RWKV_SIZES = (RWKV_WIDTH, RWKV_WIDTH, RWKV_WIDTH, DECAY_LORA, DECAY_LORA, AAA_LORA, AAA_LORA, GATE_LORA)
RWKV_COLS = 3 * RWKV_WIDTH + 2 * DECAY_LORA + 2 * AAA_LORA + GATE_LORA
GATE_COLS = 2 * D_MODEL
IN_COLS = NA_COLS + RWKV_COLS + GATE_COLS
PEER_HEADS = 8
PEER_DQ = 256
PEER_N_KEYS = 128
PEER_N_EXPERTS = PEER_N_KEYS * PEER_N_KEYS
PEER_TOPK = 16
PEER_CHUNK = 128
NORM_EPS = 1e-6

kernel_name = "hybrid_na_rwkv7_peer_block"


def _split(z, sizes):
    parts, start = [], 0
    for s in sizes:
        parts.append(z[..., start:start + s])
        start += s
    return parts


def rms_norm(x, w):
    xf = x.astype(jnp.float32)
    y = xf * lax.rsqrt(jnp.mean(xf * xf, axis=-1, keepdims=True) + NORM_EPS)
    return y.astype(x.dtype) * w


def neighbourhood_attention(q, k, v, rpb):
    B, S, H, Dh = q.shape
    rows = S // GRID_W
    kh = min(NA_KH, rows)
    q_g = q.reshape(B, rows, GRID_W, H, Dh)
    k_g = k.reshape(B, rows, GRID_W, H, Dh)
    v_g = v.reshape(B, rows, GRID_W, H, Dh)
    row_q = jnp.arange(rows)
    row_start = jnp.clip(row_q - kh // 2, 0, rows - kh)
    row_idx = row_start[:, None] + jnp.arange(kh)
    k_band = k_g[:, row_idx]
    v_band = v_g[:, row_idx]
    logits = jnp.einsum('biqhd,birkhd->bhiqrk', q_g, k_band).astype(jnp.float32) * (Dh ** -0.5)
    col = jnp.arange(GRID_W)
    col_start = jnp.clip(col - NA_KW // 2, 0, GRID_W - NA_KW)
    col_mask = (col[None, :] >= col_start[:, None]) & (col[None, :] < col_start[:, None] + NA_KW)
    dr = row_idx - row_q[:, None] + NA_KH - 1
    dc = jnp.clip(col[None, :] - col[:, None], -(NA_KW - 1), NA_KW - 1) + NA_KW - 1
    bias = rpb[:, dr[:, None, :, None], dc[None, :, None, :]]
    logits = jnp.where(col_mask[:, None, :], logits + bias.astype(jnp.float32)[None], -jnp.inf)
    probs = jax.nn.softmax(logits.reshape(B, H, rows, GRID_W, kh * GRID_W), axis=-1)
    probs = probs.reshape(B, H, rows, GRID_W, kh, GRID_W).astype(v.dtype)
    out = jnp.einsum('bhiqrk,birkhd->biqhd', probs, v_band)
    return out.reshape(B, S, H * Dh)


def centred_token_shift(y, mu):
    prev = jnp.pad(y[:, :-1], ((0, 0), (1, 0), (0, 0)))
    nxt = jnp.pad(y[:, 1:], ((0, 0), (0, 1), (0, 0)))
    return y + mu[0] * (prev - y) + mu[1] * (nxt - y)


def rwkv7_bidirectional(zb, mu, w0, w2, a0, a2, g2, k_k, k_a, r_k, ln_w, ln_b):
    B, S, _ = zb.shape
    H, N, C = RWKV_HEADS, RWKV_HEAD_DIM, RWKV_WIDTH
    f32 = jnp.float32
    zb = centred_token_shift(zb, mu)
    r, k, v, wd_f, wd_b, ad_f, ad_b, gd = _split(zb, RWKV_SIZES)
    wd = jnp.stack([wd_f, wd_b])
    ad = jnp.stack([ad_f, ad_b])
    w_logit = w0[:, None, None, :] + jnp.einsum('nbsr,nrc->nbsc', jnp.tanh(wd), w2)
    decay = jnp.exp(-jnp.exp(-jax.nn.softplus(-w_logit.astype(f32)) - 0.5))
    iclr = jax.nn.sigmoid((a0[:, None, None, :] + jnp.einsum('nbsr,nrc->nbsc', ad, a2)).astype(f32))
    g = (jax.nn.sigmoid(gd) @ g2).astype(f32)
    rf, kf, vf = r.astype(f32), k.astype(f32), v.astype(f32)
    kk = (kf * k_k.astype(f32)).reshape(B, S, H, N)
    kk = kk / jnp.maximum(jnp.sqrt(jnp.sum(kk * kk, axis=-1, keepdims=True)), 1e-12)
    kk = kk.reshape(B, S, C)
    k_dir = kf[None] * (1.0 + (iclr - 1.0) * k_a.astype(f32))
    b_dir = kk[None] * iclr

    def directional(t):
        t = jnp.stack([t[0], jnp.flip(t[1], axis=1)])
        return jnp.moveaxis(t.reshape(2, B, S, H, N), 2, 0)

    def shared(t):
        return directional(jnp.stack([t, t]))

    xs = (shared(rf), directional(decay), directional(k_dir), shared(vf), shared(-kk), directional(b_dir))

    def step(state, inp):
        r_t, w_t, k_t, v_t, negkk_t, b_t = inp
        sa = jnp.einsum('nbhvk,nbhk->nbhv', state, negkk_t)
        state = state * w_t[..., None, :] + sa[..., :, None] * b_t[..., None, :] + v_t[..., :, None] * k_t[..., None, :]
        return state, jnp.einsum('nbhvk,nbhk->nbhv', state, r_t)

    state0 = jnp.zeros((2, B, H, N, N), f32)
    _, o = lax.scan(step, state0, xs)
    o = jnp.moveaxis(o[:, 0] + jnp.flip(o[:, 1], axis=0), 0, 1)
    mean = jnp.mean(o, axis=-1, keepdims=True)
    var = jnp.mean(jnp.square(o - mean), axis=-1, keepdims=True)
    o = ((o - mean) * lax.rsqrt(var + RWKV_GN_EPS)).reshape(B, S, C) * ln_w.astype(f32) + ln_b.astype(f32)
    bonus = jnp.sum(rf.reshape(B, S, H, N) * kf.reshape(B, S, H, N) * r_k.astype(f32), axis=-1, keepdims=True) * vf.reshape(B, S, H, N)
    y = (o + bonus.reshape(B, S, C)) * g
    return y.astype(zb.dtype)


def peer_ffn(u, w_q, sub_k1, sub_k2, exp_u, exp_v):
    B, S, D = u.shape
    T = B * S
    half = PEER_DQ // 2
    q = (u @ w_q).reshape(T, PEER_HEADS, PEER_DQ)
    s1 = jnp.einsum('thd,nd->thn', q[..., :half], sub_k1).astype(jnp.float32)
    s2 = jnp.einsum('thd,nd->thn', q[..., half:], sub_k2).astype(jnp.float32)
    v1, i1 = lax.top_k(s1, PEER_TOPK)
    v2, i2 = lax.top_k(s2, PEER_TOPK)
    cand_s = (v1[..., :, None] + v2[..., None, :]).reshape(T, PEER_HEADS, PEER_TOPK * PEER_TOPK)
    cand_i = (i1[..., :, None] * PEER_N_KEYS + i2[..., None, :]).reshape(T, PEER_HEADS, PEER_TOPK * PEER_TOPK)
    top_s, pos = lax.top_k(cand_s, PEER_TOPK)
    expert_idx = jnp.take_along_axis(cand_i, pos, axis=-1)
    gate = jax.nn.softmax(top_s, axis=-1).astype(u.dtype)
    n_chunks = T // PEER_CHUNK

    def chunk(args):
        xc, ec, gc = args
        hidden = jax.nn.gelu(jnp.einsum('cd,chkd->chk', xc, exp_u[ec]), approximate=False) * gc
        return jnp.einsum('chk,chkd->cd', hidden, exp_v[ec])

    out = lax.map(chunk, (u.reshape(n_chunks, PEER_CHUNK, D),
                          expert_idx.reshape(n_chunks, PEER_CHUNK, PEER_HEADS, PEER_TOPK),
                          gate.reshape(n_chunks, PEER_CHUNK, PEER_HEADS, PEER_TOPK)))
    return out.reshape(B, S, D)


def hybrid_block(x, c, ada_w, ada_b, norm1_w, norm2_w, w_in, b_gate, na_q_norm_w, na_k_norm_w, na_rpb,
                 rwkv_mu, rwkv_w0, rwkv_w2, rwkv_a0, rwkv_a2, rwkv_g2, rwkv_k_k, rwkv_k_a, rwkv_r_k,
                 rwkv_ln_w, rwkv_ln_b, w_branch_a, w_branch_b, w_out, peer_w_q, peer_sub_k1, peer_sub_k2,
                 peer_u, peer_v):
    B, S, D = x.shape
    mod = jax.nn.silu(c) @ ada_w + ada_b
    shift1, scale1, gate1, shift2, scale2, gate2 = jnp.split(mod[:, None, :], 6, axis=-1)
    u = rms_norm(x, norm1_w) * (1.0 + scale1) + shift1
    z = u @ w_in
    za, zb, zg = _split(z, (NA_COLS, RWKV_COLS, GATE_COLS))
    q, k, v = jnp.split(za, 3, axis=-1)
    q = rms_norm(q.reshape(B, S, NA_HEADS, NA_HEAD_DIM), na_q_norm_w)
    k = rms_norm(k.reshape(B, S, NA_HEADS, NA_HEAD_DIM), na_k_norm_w)
    v = v.reshape(B, S, NA_HEADS, NA_HEAD_DIM)
    o_a = neighbourhood_attention(q, k, v, na_rpb)
    o_b = rwkv7_bidirectional(zb, rwkv_mu, rwkv_w0, rwkv_w2, rwkv_a0, rwkv_a2, rwkv_g2,
                              rwkv_k_k, rwkv_k_a, rwkv_r_k, rwkv_ln_w, rwkv_ln_b)
    gates = jax.nn.sigmoid((zg + b_gate).astype(jnp.float32)).astype(x.dtype)
    gate_a, gate_b = jnp.split(gates, 2, axis=-1)
    merged = gate_a * (o_a @ w_branch_a) + gate_b * (o_b @ w_branch_b)
    x = x + gate1 * (merged @ w_out)
    u2 = rms_norm(x, norm2_w) * (1.0 + scale2) + shift2
    x = x + gate2 * peer_ffn(u2, peer_w_q, peer_sub_k1, peer_sub_k2, peer_u, peer_v)
    return x


def setup_inputs(seed: int = 0) -> dict:
    key = jax.random.key(seed)
    ks = iter(jax.random.split(key, 40))
    L = DEPTH

    def nrm(shape, scale):
        return scale * jax.random.normal(next(ks), shape, jnp.float32)

    def unif(shape, lo, hi):
        return jax.random.uniform(next(ks), shape, jnp.float32, minval=lo, maxval=hi)

    return {
        "x": nrm((BATCH, SEQ, D_MODEL), 1.0),
        "c": nrm((BATCH, D_MODEL), 1.0),
        "ada_w": nrm((L, D_MODEL, 6 * D_MODEL), 0.5 * D_MODEL ** -0.5),
        "ada_b": nrm((L, 6 * D_MODEL), 0.01),
        "norm1_w": 1.0 + nrm((L, D_MODEL), 0.02),
        "norm2_w": 1.0 + nrm((L, D_MODEL), 0.02),
        "w_in": nrm((L, D_MODEL, IN_COLS), D_MODEL ** -0.5),
        "b_gate": nrm((L, GATE_COLS), 0.1),
        "na_q_norm_w": 1.0 + nrm((L, NA_HEAD_DIM), 0.02),
        "na_k_norm_w": 1.0 + nrm((L, NA_HEAD_DIM), 0.02),
        "na_rpb": nrm((L, NA_HEADS, 2 * NA_KH - 1, 2 * NA_KW - 1), 0.1),
        "rwkv_mu": unif((L, 2, RWKV_COLS), 0.0, 0.5),
        "rwkv_w0": unif((L, 2, RWKV_WIDTH), -3.0, 1.0),
        "rwkv_w2": nrm((L, 2, DECAY_LORA, RWKV_WIDTH), DECAY_LORA ** -0.5),
        "rwkv_a0": nrm((L, 2, RWKV_WIDTH), 0.5),
        "rwkv_a2": nrm((L, 2, AAA_LORA, RWKV_WIDTH), AAA_LORA ** -0.5),
        "rwkv_g2": nrm((L, GATE_LORA, RWKV_WIDTH), GATE_LORA ** -0.5),
        "rwkv_k_k": 0.85 + nrm((L, RWKV_WIDTH), 0.02),
        "rwkv_k_a": 1.0 + nrm((L, RWKV_WIDTH), 0.02),
        "rwkv_r_k": nrm((L, RWKV_HEADS, RWKV_HEAD_DIM), 0.1),
        "rwkv_ln_w": 1.0 + nrm((L, RWKV_WIDTH), 0.02),
        "rwkv_ln_b": nrm((L, RWKV_WIDTH), 0.01),
        "w_branch_a": nrm((L, NA_WIDTH, D_MODEL), NA_WIDTH ** -0.5),
        "w_branch_b": nrm((L, RWKV_WIDTH, D_MODEL), RWKV_WIDTH ** -0.5),
        "w_out": nrm((L, D_MODEL, D_MODEL), D_MODEL ** -0.5),
        "peer_w_q": nrm((L, D_MODEL, PEER_HEADS * PEER_DQ), D_MODEL ** -0.5),
        "peer_sub_k1": nrm((L, PEER_N_KEYS, PEER_DQ // 2), (PEER_DQ // 2) ** -0.5),
        "peer_sub_k2": nrm((L, PEER_N_KEYS, PEER_DQ // 2), (PEER_DQ // 2) ** -0.5),
        "peer_u": nrm((L, PEER_N_EXPERTS, D_MODEL), D_MODEL ** -0.5),
        "peer_v": nrm((L, PEER_N_EXPERTS, D_MODEL), 0.5),
    }


def reference(x, c, ada_w, ada_b, norm1_w, norm2_w, w_in, b_gate, na_q_norm_w, na_k_norm_w, na_rpb,
              rwkv_mu, rwkv_w0, rwkv_w2, rwkv_a0, rwkv_a2, rwkv_g2, rwkv_k_k, rwkv_k_a, rwkv_r_k,
              rwkv_ln_w, rwkv_ln_b, w_branch_a, w_branch_b, w_out, peer_w_q, peer_sub_k1, peer_sub_k2,
              peer_u, peer_v):
    h = x
    for layer in range(DEPTH):
        h = hybrid_block(h, c, ada_w[layer], ada_b[layer], norm1_w[layer], norm2_w[layer], w_in[layer],
                         b_gate[layer], na_q_norm_w[layer], na_k_norm_w[layer], na_rpb[layer],
                         rwkv_mu[layer], rwkv_w0[layer], rwkv_w2[layer], rwkv_a0[layer], rwkv_a2[layer],
                         rwkv_g2[layer], rwkv_k_k[layer], rwkv_k_a[layer], rwkv_r_k[layer],
                         rwkv_ln_w[layer], rwkv_ln_b[layer], w_branch_a[layer], w_branch_b[layer],
                         w_out[layer], peer_w_q[layer], peer_sub_k1[layer], peer_sub_k2[layer],
                         peer_u[layer], peer_v[layer])
    return h
```

```python
import contextlib
import numpy as np
import concourse.bass as bass
import concourse.mybir as mybir

F32 = mybir.dt.float32
BF16 = mybir.dt.bfloat16
I32 = mybir.dt.int32
ALU = mybir.AluOpType
AF = mybir.ActivationFunctionType
AX = mybir.AxisListType

ENGS = ("pe", "act", "dve", "pool", "sp")
N_DMA_SEMS = 48
N_SW_SEMS = 16


class Prog:
    def __init__(self, nc, stack, same_engine_sync=True):
        self.nc = nc
        self.same_engine_sync = same_engine_sync
        self.esem = {e: stack.enter_context(nc.semaphore("se_" + e)) for e in ENGS}
        self.dsem = [stack.enter_context(nc.semaphore("sd_%d" % i)) for i in range(N_DMA_SEMS)]
        self.ecnt = {e: 0 for e in ENGS}
        self.dcnt = [0] * N_DMA_SEMS
        self.waited = {e: {} for e in ENGS}
        self._reset_phase()
        self.n_ops = 0

    def _reset_phase(self):
        self.ops = []
        self.last_w = {}
        self.readers = {}
        self.dkey = {}

    def _deps(self, reads, writes):
        deps = []
        for b in reads:
            t = self.last_w.get(b)
            if t is not None:
                deps.append(t)
        for b in writes:
            t = self.last_w.get(b)
            if t is not None:
                deps.append(t)
            deps.extend(self.readers.get(b, ()))
        return deps

    def _commit(self, tok, reads, writes):
        for b in reads:
            self.readers.setdefault(b, []).append(tok)
        for b in writes:
            self.last_w[b] = tok
            self.readers[b] = []

    def op(self, eng, fn, reads=(), writes=()):
        deps = self._deps(reads, writes)
        self.ecnt[eng] += 1
        tok = (("e", eng), self.ecnt[eng], eng)
        self.ops.append((eng, fn, deps, ("e", eng), 1))
        self._commit(tok, reads, writes)
        return tok

    def dma(self, eng, fn, semkey, reads=(), writes=()):
        deps = self._deps(reads, writes)
        sw = (eng == "pool")
        semkey = (sw, semkey)
        if semkey not in self.dkey:
            used = [v for k_, v in self.dkey.items() if k_[0] == sw]
            lo, hi = (0, N_SW_SEMS) if sw else (N_SW_SEMS, N_DMA_SEMS)
            nxt = lo + len(used)
            assert nxt < hi, "out of dma sems"
            self.dkey[semkey] = nxt
        i = self.dkey[semkey]
        self.dcnt[i] += 16
        tok = (("d", i), self.dcnt[i], "dma")
        self.ops.append((eng, fn, deps, ("d", i), 16))
        self._commit(tok, reads, writes)
        return tok

    def _sem(self, k):
        return self.esem[k[1]] if k[0] == "e" else self.dsem[k[1]]

    def flush(self):
        nc = self.nc
        per_eng = {e: [] for e in ENGS}
        for o in self.ops:
            per_eng[o[0]].append(o)
        self.n_ops += len(self.ops)
        finals = [(("e", e), self.ecnt[e]) for e in ENGS] + [(("d", i), self.dcnt[i]) for i in range(N_DMA_SEMS)]
        finals = [(k, v) for k, v in finals if v > 0]

        def make(ename):
            def body(eng):
                waited = self.waited[ename]
                for (_, fn, deps, inck, incv) in per_eng[ename]:
                    need = {}
                    for (k, v, src) in deps:
                        if src == ename and not self.same_engine_sync:
                            continue
                        if waited.get(k, 0) >= v:
                            continue
                        if need.get(k, 0) < v:
                            need[k] = v
                    for k, v in need.items():
                        eng.wait_ge(self._sem(k), v)
                        waited[k] = v
                    ins = fn(eng)
                    ins.then_inc(self._sem(inck), incv)
                for k, v in finals:
                    if waited.get(k, 0) < v:
                        eng.wait_ge(self._sem(k), v)
                        waited[k] = v
            return body

        with nc.Block() as block:
            block.tensor(make("pe"))
            block.scalar(make("act"))
            block.vector(make("dve"))
            block.gpsimd(make("pool"))
            block.sync(make("sp"))
        self._reset_phase()


import contextlib
import numpy as np

D = 2048
T = 16384
S = 8192
EPS = 1e-6
GN_EPS = 64e-5
DEC = 0.6065306597126334
A_R, A_V, A_A, A_LWF, A_KF, A_BF, A_LWB, A_KB, A_BB, A_G, A_BON, A_OF, A_OB = range(13)


def build_A(nc, stage="all", dbg=False, nblk=32):
    dt = nc.dram_tensor
    x = dt("x", [T, D], F32, kind="ExternalInput").ap()
    c2 = dt("c2", [2, D], F32, kind="ExternalInput").ap()
    ada_w = dt("ada_w", [D, 6 * D], F32, kind="ExternalInput").ap()
    ada_b = dt("ada_b", [6 * D], F32, kind="ExternalInput").ap()
    n1w = dt("norm1_w", [D], F32, kind="ExternalInput").ap()
    w_rw = dt("w_rw", [D, 1024], F32, kind="ExternalInput").ap()
    muF_in = dt("muF", [128, 9, 2], F32, kind="ExternalInput").ap()
    chp = dt("chp", [128, 12], F32, kind="ExternalInput").ap()
    w2o = dt("w2o", [2, 96, 128], F32, kind="ExternalInput").ap()
    a2o = dt("a2o", [2, 96, 128], F32, kind="ExternalInput").ap()
    g2o = dt("g2o", [256, 128], F32, kind="ExternalInput").ap()
    masks = dt("masks", [128, 5, 256], F32, kind="ExternalInput").ap()
    obT = dt("obT_out", [128, T], F32, kind="ExternalOutput").ap()
    SC = dt("SC", [13, 128, T], F32, kind="Internal").ap()
    if dbg:
        d_sc = dt("d_sc", [13, 128, 1024], F32, kind="ExternalOutput").ap()

    with contextlib.ExitStack() as top:
        P = Prog(nc, top)
        uid = [0]

        def sbt(st, name, shape, dtp):
            uid[0] += 1
            return st.enter_context(nc.sbuf_tensor("%s_u%d" % (name, uid[0]), shape, dtp))

        def pst(st, name, shape, dtp):
            uid[0] += 1
            return st.enter_context(nc.psum_tensor("%s_u%d" % (name, uid[0]), shape, dtp))

        ident = sbt(top, "ident", [128, 128], BF16)
        identf = sbt(top, "identf", [128, 128], F32)
        BO = sbt(top, "BO", [128, 128], BF16)
        BOf = sbt(top, "BOf", [128, 128], F32)
        msk = sbt(top, "msk", [128, 5, 256], F32)
        chs = sbt(top, "chs", [128, 12], F32)
        modF = sbt(top, "modF", [128, 2, 16, 2], F32)
        m1F = sbt(top, "m1F", [128, 16, 2], F32)
        P.op("pool", lambda e: e.memset(identf[:], 0.0), writes=["identf"])
        P.op("pool", lambda e: e.affine_select(out=identf[:], in_=identf[:], pattern=[[-1, 128]],
                                               compare_op=ALU.not_equal, fill=1.0, base=0, channel_multiplier=1),
             reads=["identf"], writes=["identf"])
        P.op("dve", lambda e: e.tensor_copy(out=ident[:], in_=identf[:]), reads=["identf"], writes=["ident"])
        P.dma("sp", lambda e: e.dma_start(out=msk[:], in_=masks[:, :, :]), "msk", writes=["msk"])
        P.dma("sp", lambda e: e.dma_start(out=chs[:], in_=chp[:, :]), "chs", writes=["chs"])
        P.op("dve", lambda e: e.tensor_copy(out=BO[:], in_=msk[:, 4, 0:128]), reads=["msk"], writes=["BO"])
        P.op("dve", lambda e: e.tensor_copy(out=BOf[:], in_=msk[:, 4, 0:128]), reads=["msk"], writes=["BOf"])

        with contextlib.ExitStack() as st:
            cT = sbt(st, "cT", [128, 16, 2], F32)
            scT = sbt(st, "scT", [128, 16, 2], BF16)
            abF = sbt(st, "abF", [128, 2, 16], F32)
            nwF = sbt(st, "nwF", [128, 16], F32)
            awb = [sbt(st, "awb%d" % i, [128, 16, 512], BF16) for i in range(2)]
            pmod = pst(st, "pmod", [128, 512], F32)
            for b in range(2):
                P.dma("sp", lambda e, b=b: e.dma_start(out=cT[:, :, b], in_=c2[b].rearrange("(c p) -> p c", p=128), allow_slow_non_contiguous=True), "cT", writes=[("cT", b)])
            P.dma("sp", lambda e: e.dma_start(out=abF[:], in_=ada_b[0:2 * D].rearrange("(s c p) -> p s c", p=128, c=16), allow_slow_non_contiguous=True), "abF", writes=["abF"])
            P.dma("sp", lambda e: e.dma_start(out=nwF[:], in_=n1w.rearrange("(c p) -> p c", p=128), allow_slow_non_contiguous=True), "nwF", writes=["nwF"])
            P.op("act", lambda e: e.activation(out=scT[:], in_=cT[:], func=AF.Silu), reads=[("cT", 0), ("cT", 1)], writes=["scT"])
            awr = ada_w.rearrange("(kc p) n -> p kc n", p=128)
            blk = 0
            for seg in range(2):
                for nb in range(4):
                    buf = awb[blk % 2]; bk = "awb%d" % (blk % 2); blk += 1
                    col0 = seg * D + nb * 512
                    P.dma("pool", lambda e, buf=buf, col0=col0: e.dma_start(out=buf[:], in_=awr[:, :, col0:col0 + 512]), bk, writes=[bk])
                    for j in range(4):
                        n = nb * 4 + j
                        for kc in range(16):
                            P.op("pe", lambda e, buf=buf, kc=kc, j=j, n=n: e.matmul(pmod[:, 2 * n:2 * n + 2], lhsT=buf[:, kc, j * 128:(j + 1) * 128],
                                                                                   rhs=scT[:, kc, :], start=(kc == 0), stop=(kc == 15)),
                                 reads=[bk, "scT"], writes=["pmod"])
                    if nb == 3:
                        P.op("dve", lambda e, seg=seg: e.tensor_tensor(out=modF[:, seg], in0=pmod[:, 0:32].rearrange("p (c b) -> p c b", b=2),
                                                                       in1=abF[:, seg, :, None].to_broadcast([128, 16, 2]), op=ALU.add),
                             reads=["pmod", "abF"], writes=[("modF", seg)])
            P.op("dve", lambda e: e.scalar_tensor_tensor(out=m1F[:], in0=modF[:, 1], scalar=1.0, in1=nwF[:, :, None].to_broadcast([128, 16, 2]),
                                                         op0=ALU.add, op1=ALU.mult), reads=[("modF", 1), "nwF"], writes=["m1F"])
            P.flush()

        with contextlib.ExitStack() as st:
            Wr = sbt(st, "Wr", [128, 16, 1024], BF16)
            muF = sbt(st, "muF", [128, 9, 2], F32)
            w2s = sbt(st, "w2s", [96, 2, 128], BF16)
            a2s = sbt(st, "a2s", [96, 2, 128], BF16)
            g2s = sbt(st, "g2s", [128, 2, 128], BF16)
            xts = [sbt(st, "xt%d" % i, [128, D], F32) for i in range(2)]
            ss = sbt(st, "ss", [128, 1], F32)
            rstd = sbt(st, "rstd", [128, 1], F32)
            xn = sbt(st, "xn", [128, D], BF16)
            nt_tmp = sbt(st, "nt_tmp", [128, 512], F32)
            uTb = [sbt(st, "uTb%d" % i, [128, 16, 512], BF16) for i in range(2)]
            zb = [sbt(st, "zb%d" % i, [128, 9, 514], F32) for i in range(2)]
            d1 = sbt(st, "d1", [128, 9, 512], F32)
            d2 = sbt(st, "d2", [128, 9, 512], F32)
            ZS = d1
            th = sbt(st, "th", [96, 2, 512], BF16)
            adb = sbt(st, "adb", [96, 2, 512], BF16)
            sg = sbt(st, "sg", [128, 2, 512], BF16)
            outs = {k_: sbt(st, "o_%d" % k_, [128, 512], F32) for k_ in (A_A, A_LWF, A_KF, A_BF, A_LWB, A_KB, A_BB, A_G, A_BON)}
            icl = [sbt(st, "icl%d" % i, [128, 512], F32) for i in range(2)]
            kk = sbt(st, "kk", [128, 512], F32)
            kk2 = sbt(st, "kk2", [128, 512], BF16)
            nrm = sbt(st, "nrm", [128, 512], F32)
            tt_ = sbt(st, "tt_", [128, 512], F32)
            rk = sbt(st, "rk", [128, 512], BF16)
            c1mka = sbt(st, "c1mka", [128, 1], F32)
            ptr = pst(st, "ptr", [128, 1024], BF16)
            pz = [pst(st, "pz%d" % i, [128, 512], F32) for i in range(3)]
            pl = [pst(st, "pl%d" % i, [128, 512], F32) for i in range(3)]
            P.dma("pool", lambda e: e.dma_start(out=Wr[:], in_=w_rw.rearrange("(kc p) n -> p kc n", p=128)), "Wr", writes=["Wr"])
            P.dma("sp", lambda e: e.dma_start(out=muF[:], in_=muF_in[:, :, :]), "muF", writes=["muF"])
            P.dma("pool", lambda e: e.dma_start(out=w2s[:], in_=w2o.rearrange("n r c -> r n c")), "w2s", writes=["w2s"])
            P.dma("pool", lambda e: e.dma_start(out=a2s[:], in_=a2o.rearrange("n r c -> r n c")), "a2s", writes=["a2s"])
            P.dma("pool", lambda e: e.dma_start(out=g2s[:], in_=g2o.rearrange("(h r) c -> r h c", h=2)), "g2s", writes=["g2s"])
            P.op("pool", lambda e: e.memset(zb[0][:], 0.0), writes=["zb0"])
            P.op("pool", lambda e: e.memset(zb[1][:], 0.0), writes=["zb1"])
            P.op("dve", lambda e: e.tensor_scalar(out=c1mka[:], in0=chs[:, 5:6], scalar1=-1.0, scalar2=1.0, op0=ALU.mult, op1=ALU.add), reads=["chs"], writes=["c1mka"])
            GROUPS = [(0, 128, 128), (128, 128, 128), (256, 128, 128), (384, 96, 96), (480, 96, 96), (576, 96, 96), (672, 96, 96), (768, 128, 128), (896, 128, 128)]
            NPB = 16 * (nblk // 2) // 16 if False else None

            def norm_tile(xt_key, xt, b, dst, pos, dstkey):
                P.op("act", lambda e: e.activation(out=xn[:], in_=xt[:], func=AF.Square, accum_out=ss[:]), reads=[xt_key], writes=["xn", "ss"])
                P.op("dve", lambda e: e.tensor_scalar(out=rstd[:], in0=ss[:], scalar1=1.0 / D, scalar2=EPS, op0=ALU.mult, op1=ALU.add), reads=["ss"], writes=["rstd"])
                P.op("dve", lambda e: e.reciprocal(out=rstd[:], in_=rstd[:]), reads=["rstd"], writes=["rstd"])
                P.op("act", lambda e: e.activation(out=rstd[:], in_=rstd[:], func=AF.Sqrt), reads=["rstd"], writes=["rstd"])
                P.op("dve", lambda e: e.tensor_scalar(out=xn[:], in0=xt[:], scalar1=rstd[:, 0:1], scalar2=None, op0=ALU.mult), reads=[xt_key, "rstd"], writes=["xn"])
                for g4 in range(4):
                    for j in range(4):
                        c = g4 * 4 + j
                        P.op("pe", lambda e, c=c, j=j: e.transpose(out=ptr[:, j * 128:(j + 1) * 128], in_=xn[:, c * 128:(c + 1) * 128], identity=ident[:]),
                             reads=["xn", "ident"], writes=["ptr"])
                    P.op("dve", lambda e, g4=g4: e.tensor_tensor(out=nt_tmp[:].rearrange("p (a b) -> p a b", a=4),
                                                                in0=ptr[:, 0:512].rearrange("p (a b) -> p a b", a=4),
                                                                in1=m1F[:, g4 * 4:(g4 + 1) * 4, b:b + 1].to_broadcast([128, 4, 128]), op=ALU.mult),
                         reads=["ptr", "m1F"], writes=["nt_tmp"])
                    P.op("pool", lambda e, g4=g4: e.tensor_tensor(out=dst[:, g4 * 4:(g4 + 1) * 4, pos:pos + 128],
                                                                 in0=nt_tmp[:].rearrange("p (a b) -> p a b", a=4),
                                                                 in1=modF[:, 0, g4 * 4:(g4 + 1) * 4, b:b + 1].to_broadcast([128, 4, 128]), op=ALU.add),
                         reads=["nt_tmp", ("modF", 0)], writes=[dstkey])

            def finalize(m):
                z = zb[m % 2]; zk = "zb%d" % (m % 2)
                zc = z[:, :, 1:513]
                P.op("pool", lambda e: e.tensor_tensor(out=d1[:], in0=z[:, :, 0:512], in1=zc, op=ALU.subtract), reads=[zk], writes=["d1", "ZS"])
                P.op("dve", lambda e: e.tensor_tensor(out=d2[:], in0=z[:, :, 2:514], in1=zc, op=ALU.subtract), reads=[zk], writes=["d2"])
                P.op("pool", lambda e: e.tensor_tensor(out=d1[:], in0=d1[:], in1=muF[:, :, 0:1].to_broadcast([128, 9, 512]), op=ALU.mult), reads=["d1", "muF"], writes=["d1"])
                P.op("dve", lambda e: e.tensor_tensor(out=d2[:], in0=d2[:], in1=muF[:, :, 1:2].to_broadcast([128, 9, 512]), op=ALU.mult), reads=["d2", "muF"], writes=["d2"])
                P.op("pool", lambda e: e.tensor_tensor(out=d1[:], in0=d1[:], in1=zc, op=ALU.add), reads=["d1", zk], writes=["d1"])
                P.op("dve", lambda e: e.tensor_tensor(out=ZS[:], in0=d1[:], in1=d2[:], op=ALU.add), reads=["d1", "d2"], writes=["ZS", "d1"])
                rS, kS, vS = ZS[:, 0, :], ZS[:, 1, :], ZS[:, 2, :]
                P.op("act", lambda e: e.activation(out=th[:], in_=ZS[0:96, 3:5, :], func=AF.Tanh), reads=["ZS"], writes=["th"])
                P.op("pool", lambda e: e.tensor_copy(out=adb[:], in_=ZS[0:96, 5:7, :]), reads=["ZS"], writes=["adb"])
                P.op("act", lambda e: e.activation(out=sg[:], in_=ZS[:, 7:9, :], func=AF.Sigmoid), reads=["ZS"], writes=["sg"])
                for d_ in range(2):
                    pw = pl[d_]; pwk = "pl%d" % d_
                    P.op("pe", lambda e, pw=pw, d_=d_: e.matmul(pw[:], lhsT=w2s[:, d_, :], rhs=th[:, d_, :], start=True, stop=True), reads=["w2s", "th"], writes=[pwk])
                    lw = outs[A_LWF if d_ == 0 else A_LWB]; lwk = ("o", A_LWF if d_ == 0 else A_LWB)
                    P.op("act", lambda e, pw=pw, lw=lw, d_=d_: e.activation(out=lw[:], in_=pw[:], func=AF.Sigmoid, bias=chs[:, d_:d_ + 1]), reads=[pwk, "chs"], writes=[lwk])
                    P.op("pool", lambda e, lw=lw: e.tensor_scalar(out=lw[:], in0=lw[:], scalar1=-DEC, scalar2=None, op0=ALU.mult), reads=[lwk], writes=[lwk])
                    P.op("pe", lambda e, pw=pw, d_=d_: e.matmul(pw[:], lhsT=a2s[:, d_, :], rhs=adb[:, d_, :], start=True, stop=True), reads=["a2s", "adb"], writes=[pwk])
                    P.op("act", lambda e, pw=pw, d_=d_: e.activation(out=icl[d_][:], in_=pw[:], func=AF.Sigmoid, bias=chs[:, 2 + d_:3 + d_]), reads=[pwk, "chs"], writes=["icl%d" % d_])
                pg_ = pl[2]
                P.op("pe", lambda e: e.matmul(pg_[:], lhsT=g2s[:, 0, :], rhs=sg[:, 0, :], start=True, stop=False), reads=["g2s", "sg"], writes=["pl2"])
                P.op("pe", lambda e: e.matmul(pg_[:], lhsT=g2s[:, 1, :], rhs=sg[:, 1, :], start=False, stop=True), reads=["g2s", "sg"], writes=["pl2"])
                P.op("act", lambda e: e.copy(out=outs[A_G][:], in_=pg_[:]), reads=["pl2"], writes=[("o", A_G)])
                P.op("dve", lambda e: e.tensor_scalar(out=kk[:], in0=kS, scalar1=chs[:, 4:5], scalar2=None, op0=ALU.mult), reads=["ZS", "chs"], writes=["kk"])
                P.op("pool", lambda e: e.tensor_tensor(out=kk2[:], in0=kk[:], in1=kk[:], op=ALU.mult), reads=["kk"], writes=["kk2"])
                P.op("pe", lambda e: e.matmul(pl[0][:], lhsT=BO[:], rhs=kk2[:], start=True, stop=True), reads=["BO", "kk2"], writes=["pl0"])
                P.op("act", lambda e: e.activation(out=nrm[:], in_=pl[0][:], func=AF.Sqrt), reads=["pl0"], writes=["nrm"])
                P.op("dve", lambda e: e.tensor_scalar(out=nrm[:], in0=nrm[:], scalar1=1e-12, scalar2=None, op0=ALU.max), reads=["nrm"], writes=["nrm"])
                P.op("dve", lambda e: e.reciprocal(out=nrm[:], in_=nrm[:]), reads=["nrm"], writes=["nrm"])
                P.op("dve", lambda e: e.tensor_tensor(out=kk[:], in0=kk[:], in1=nrm[:], op=ALU.mult), reads=["kk", "nrm"], writes=["kk"])
                P.op("pool", lambda e: e.tensor_scalar(out=outs[A_A][:], in0=kk[:], scalar1=-1.0, scalar2=None, op0=ALU.mult), reads=["kk"], writes=[("o", A_A)])
                for d_ in range(2):
                    kd = outs[A_KF if d_ == 0 else A_KB]; bd = outs[A_BF if d_ == 0 else A_BB]
                    P.op("dve", lambda e, d_=d_: e.tensor_scalar(out=tt_[:], in0=icl[d_][:], scalar1=chs[:, 5:6], scalar2=c1mka[:, 0:1], op0=ALU.mult, op1=ALU.add),
                         reads=["icl%d" % d_, "chs", "c1mka"], writes=["tt_"])
                    P.op("dve", lambda e, kd=kd: e.tensor_tensor(out=kd[:], in0=tt_[:], in1=kS, op=ALU.mult), reads=["tt_", "ZS"], writes=[("o", A_KF if d_ == 0 else A_KB)])
                    P.op("pool", lambda e, bd=bd, d_=d_: e.tensor_tensor(out=bd[:], in0=kk[:], in1=icl[d_][:], op=ALU.mult), reads=["kk", "icl%d" % d_],
                         writes=[("o", A_BF if d_ == 0 else A_BB)])
                P.op("dve", lambda e: e.scalar_tensor_tensor(out=rk[:], in0=rS, scalar=chs[:, 6:7], in1=kS, op0=ALU.mult, op1=ALU.mult), reads=["ZS", "chs"], writes=["rk"])
                P.op("pe", lambda e: e.matmul(pl[1][:], lhsT=BO[:], rhs=rk[:], start=True, stop=True), reads=["BO", "rk"], writes=["pl1"])
                P.op("dve", lambda e: e.tensor_tensor(out=outs[A_BON][:], in0=pl[1][:], in1=vS, op=ALU.mult), reads=["pl1", "ZS"], writes=[("o", A_BON)])
                P.dma("sp", lambda e, m=m: e.dma_start(out=SC[A_R, :, m * 512:(m + 1) * 512], in_=rS), ("osc", A_R), reads=["ZS"], writes=[("SC", A_R, m)])
                P.dma("sp", lambda e, m=m: e.dma_start(out=SC[A_V, :, m * 512:(m + 1) * 512], in_=vS), ("osc", A_V), reads=["ZS"], writes=[("SC", A_V, m)])
                for k_ in outs:
                    P.dma("sp", lambda e, k_=k_, m=m: e.dma_start(out=SC[k_, :, m * 512:(m + 1) * 512], in_=outs[k_][:]), ("osc", k_),
                          reads=[("o", k_)], writes=[("SC", k_, m)])

            nb_per_batch = 16
            for n in range(nblk):
                b = n // nb_per_batch
                ub = uTb[n % 2]; ubk = "uTb%d" % (n % 2)
                for t4 in range(4):
                    tg = n * 4 + t4
                    xt = xts[tg % 2]; xk = "xt%d" % (tg % 2)
                    P.dma("sp", lambda e, xt=xt, tg=tg: e.dma_start(out=xt[:], in_=x[tg * 128:(tg + 1) * 128, :]), xk, writes=[xk])
                    norm_tile(xk, xt, b, ub, t4 * 128, (ubk, t4))
                z = zb[n % 2]; zk = "zb%d" % (n % 2)
                for gi, (c0, M, _) in enumerate(GROUPS):
                    pp = pz[gi % 3]; ppk = "pz%d" % (gi % 3)
                    for kc in range(16):
                        P.op("pe", lambda e, pp=pp, kc=kc, c0=c0, M=M, ub=ub: e.matmul(pp[0:M, :], lhsT=Wr[:, kc, c0:c0 + M], rhs=ub[:, kc, :], start=(kc == 0), stop=(kc == 15)),
                             reads=["Wr"] + [(ubk, t4) for t4 in range(4)], writes=[ppk])
                    if gi % 2 == 0:
                        P.op("act", lambda e, pp=pp, gi=gi, M=M, z=z: e.copy(out=z[0:M, gi, 1:513], in_=pp[0:M, :]), reads=[ppk], writes=[zk])
                    else:
                        P.op("dve", lambda e, pp=pp, gi=gi, M=M, z=z: e.tensor_copy(out=z[0:M, gi, 1:513], in_=pp[0:M, :]), reads=[ppk], writes=[zk])
                first = (n % nb_per_batch == 0)
                if first:
                    P.op("pool", lambda e, z=z: e.memset(z[:, :, 0:1], 0.0), writes=[zk])
                else:
                    zp = zb[(n - 1) % 2]; zpk = "zb%d" % ((n - 1) % 2)
                    P.op("pool", lambda e, z=z, zp=zp: e.tensor_copy(out=z[:, :, 0:1], in_=zp[:, :, 512:513]), reads=[zpk], writes=[zk])
                    P.op("pool", lambda e, z=z, zp=zp: e.tensor_copy(out=zp[:, :, 513:514], in_=z[:, :, 1:2]), reads=[zk], writes=[zpk])
                    finalize(n - 1)
                if n % nb_per_batch == nb_per_batch - 1 or n == nblk - 1:
                    P.op("pool", lambda e, z=z: e.memset(z[:, :, 513:514], 0.0), writes=[zk])
                    finalize(n)
            if dbg:
                dd = sbt(st, "dd", [128, 1024], F32)
                for k_ in range(11):
                    P.dma("sp", lambda e, k_=k_: e.dma_start(out=dd[:], in_=SC[k_, :, 0:1024]), "dd", reads=[("SC", k_, 0), ("SC", k_, 1)], writes=["dd"])
                    P.dma("sp", lambda e, k_=k_: e.dma_start(out=d_sc[k_, :, :], in_=dd[:]), "dd2", reads=["dd"], writes=[("dsc", k_)])
            P.flush()
        if stage == "prep":
            return

        NST = nblk * 4 // 2
        nb128 = (nblk * 512) // 128
        nbatch = 2 if nblk == 32 else 1
        per_b = nb128 // nbatch
        with contextlib.ExitStack() as st:
            PB = [pst(st, "PB%d" % i, [128, 512], F32) for i in range(4 * nbatch)]
            epsG = sbt(st, "epsG", [128, 1], F32)
            P.op("pool", lambda e: e.memset(epsG[:], GN_EPS), writes=["epsG"])

            def nm_reg(u):
                return PB[u], 0

            def xm_reg(u):
                return PB[u], 256

            hm = [msk[:, 4, 0:1], msk[:, 4, 64:65]]
            Ihs = [sbt(st, "Ihs%d" % i, [128, 128], BF16) for i in range(2)]
            for hd in range(2):
                P.op("dve", lambda e, hd=hd: e.tensor_scalar(out=Ihs[hd][:], in0=identf[:], scalar1=hm[hd], scalar2=None, op0=ALU.mult),
                     reads=["identf", "msk"], writes=[("Ihs", hd)])
            PC = []
            for pc in range(2 * nbatch):
                d_ = {}
                for nm_, shp, dtp in (("ld1", [128, 3, 128], F32), ("ld2", [128, 3, 128], F32), ("cl", [128, 128], F32), ("tmp", [128, 128], F32),
                                      ("tmp2", [128, 128], F32), ("ex", [128, 128], F32), ("tot", [128, 2], F32), ("gC", [128, 2], F32),
                                      ("atm0", [128, 128], BF16), ("atm1", [128, 128], BF16), ("btm0", [128, 128], BF16), ("btm1", [128, 128], BF16),
                                      ("ktm0", [128, 128], BF16), ("ktm1", [128, 128], BF16), ("rt", [128, 128], BF16), ("at", [128, 128], BF16),
                                      ("bh", [128, 128], BF16), ("kh", [128, 128], BF16), ("vb", [128, 128], BF16), ("TM", [128, 4, 128], BF16),
                                      ("TMV0", [128, 128], BF16), ("TMV1", [128, 128], BF16), ("Oo", [128, 128], F32)):
                    d_[nm_] = sbt(st, "pc%d_%s" % (pc, nm_), shp, dtp)
                PC.append(d_)
            UN = []
            for u in range(4 * nbatch):
                d_ = {}
                for nm_, shp, dtp in (("NM0", [128, 256], BF16), ("NM1", [128, 256], BF16), ("Ak", [128, 128], BF16), ("X0", [128, 128], BF16), ("X1", [128, 128], BF16),
                                      ("Xc0", [128, 128], BF16), ("Xc1", [128, 128], BF16), ("Vpad", [128, 128], BF16),
                                      ("Mr", [128, 256], BF16), ("RpT", [128, 128], BF16), ("OpT", [128, 128], F32), ("GT", [128, 2, 128], BF16), ("J", [128, 2, 64], F32),
                                      ("H", [128, 128], BF16)):
                    d_[nm_] = sbt(st, "un%d_%s" % (u, nm_), shp, dtp)
                UN.append(d_)
                P.op("pool", lambda e, d_=d_: e.memset(d_["H"][:], 0.0), writes=[("H", u)])
                P.op("pool", lambda e, d_=d_: e.memset(d_["GT"][:], 0.0), writes=[("GT", u)])
                P.op("pool", lambda e, d_=d_: e.memset(d_["Vpad"][:], 0.0), writes=[("Vpad", u)])

            def pair_prep(pc, bt, dr, blk):
                c_ = PC[pc]
                tok0 = bt * S + blk * 128
                k = lambda n: (n, pc)
                P.dma("sp", lambda e: e.dma_start(out=c_["ld1"][:], in_=SC[0:3, :, tok0:tok0 + 128].rearrange("k p t -> p k t")), ("ld1", pc),
                      reads=[("SC", a_, tok0 // 512) for a_ in (A_R, A_V, A_A)], writes=[k("ld1")])
                a0 = 3 if dr == 0 else 6
                P.dma("sp", lambda e: e.dma_start(out=c_["ld2"][:], in_=SC[a0:a0 + 3, :, tok0:tok0 + 128].rearrange("k p t -> p k t")), ("ld2", pc),
                      reads=[("SC", a_, tok0 // 512) for a_ in range(a0, a0 + 3)], writes=[k("ld2")])
                r_, v_, a_ = c_["ld1"][:, 0, :], c_["ld1"][:, 1, :], c_["ld1"][:, 2, :]
                lw, kd, bd = c_["ld2"][:, 0, :], c_["ld2"][:, 1, :], c_["ld2"][:, 2, :]
                cl, tmp, tmp2, ex, tot, gC = c_["cl"], c_["tmp"], c_["tmp2"], c_["ex"], c_["tot"], c_["gC"]
                P.op("dve", lambda e: e.tensor_tensor_scan(out=cl[:], data0=msk[:, 4, 128:256], data1=lw, initial=0.0, op0=ALU.mult, op1=ALU.add),
                     reads=["msk", k("ld2")], writes=[k("cl")])
                P.op("pool", lambda e: e.tensor_copy(out=tot[:], in_=cl[:].rearrange("p (c t) -> p c t", c=2)[:, :, 63]), reads=[k("cl")], writes=[k("tot")])
                totbc = tot[:, :, None].to_broadcast([128, 2, 64])
                v3 = lambda ap: ap.rearrange("p (c t) -> p c t", c=2)
                if dr == 1:
                    P.op("dve", lambda e: e.tensor_tensor(out=tmp[:], in0=lw, in1=cl[:], op=ALU.subtract), reads=[k("ld2"), k("cl")], writes=[k("tmp")])
                    P.op("dve", lambda e: e.tensor_tensor(out=v3(cl[:]), in0=v3(tmp[:]), in1=totbc, op=ALU.add), reads=[k("tmp"), k("tot")], writes=[k("cl")])
                P.op("pool", lambda e: e.tensor_tensor(out=tmp[:], in0=cl[:], in1=lw, op=ALU.subtract), reads=[k("cl"), k("ld2")], writes=[k("tmp")])
                P.op("act", lambda e: e.activation(out=ex[:], in_=tmp[:], func=AF.Exp), reads=[k("tmp")], writes=[k("ex")])
                P.op("dve", lambda e: e.tensor_tensor(out=c_["at"][:], in0=a_, in1=ex[:], op=ALU.mult), reads=[k("ld1"), k("ex")], writes=[k("at")])
                for hd in range(2):
                    P.op("pool" if hd else "dve", lambda e, hd=hd: e.tensor_scalar(out=c_["atm%d" % hd][:], in0=c_["at"][:], scalar1=hm[hd], scalar2=None, op0=ALU.mult),
                         reads=[k("at"), "msk"], writes=[k("atm%d" % hd)])
                P.op("act", lambda e: e.activation(out=ex[:], in_=cl[:], func=AF.Exp, scale=-1.0), reads=[k("cl")], writes=[k("ex")])
                for hd in range(2):
                    P.op("dve", lambda e, hd=hd: e.scalar_tensor_tensor(out=c_["btm%d" % hd][:], in0=bd, scalar=hm[hd], in1=ex[:], op0=ALU.mult, op1=ALU.mult),
                         reads=[k("ld2"), k("ex"), "msk"], writes=[k("btm%d" % hd)])
                    P.op("dve", lambda e, hd=hd: e.scalar_tensor_tensor(out=c_["ktm%d" % hd][:], in0=kd, scalar=hm[hd], in1=ex[:], op0=ALU.mult, op1=ALU.mult),
                         reads=[k("ld2"), k("ex"), "msk"], writes=[k("ktm%d" % hd)])
                P.op("act", lambda e: e.activation(out=ex[:], in_=cl[:], func=AF.Exp), reads=[k("cl")], writes=[k("ex")])
                P.op("dve", lambda e: e.tensor_tensor(out=c_["rt"][:], in0=r_, in1=ex[:], op=ALU.mult), reads=[k("ld1"), k("ex")], writes=[k("rt")])
                P.op("pool", lambda e: e.tensor_tensor(out=v3(tmp2[:]), in0=totbc, in1=v3(cl[:]), op=ALU.subtract), reads=[k("tot"), k("cl")], writes=[k("tmp2")])
                P.op("act", lambda e: e.activation(out=ex[:], in_=tmp2[:], func=AF.Exp), reads=[k("tmp2")], writes=[k("ex")])
                P.op("dve", lambda e: e.tensor_tensor(out=c_["bh"][:], in0=bd, in1=ex[:], op=ALU.mult), reads=[k("ld2"), k("ex")], writes=[k("bh")])
                P.op("pool", lambda e: e.tensor_tensor(out=c_["kh"][:], in0=kd, in1=ex[:], op=ALU.mult), reads=[k("ld2"), k("ex")], writes=[k("kh")])
                P.op("act", lambda e: e.activation(out=gC[:], in_=tot[:], func=AF.Exp), reads=[k("tot")], writes=[k("gC")])
                P.op("pool", lambda e: e.tensor_copy(out=c_["vb"][:], in_=v_), reads=[k("ld1")], writes=[k("vb")])
                pt_ = PB[pc * 2][:, 0:256].bitcast(BF16); ptk = ("PB", pc * 2)
                for j, nm_ in enumerate(("at", "bh", "kh", "vb")):
                    P.op("pe", lambda e, j=j, nm_=nm_: e.transpose(out=pt_[:, j * 128:(j + 1) * 128], in_=c_[nm_][:], identity=ident[:]),
                         reads=[k(nm_), "ident"], writes=[ptk])
                P.op("act", lambda e: e.copy(out=c_["TM"][:], in_=pt_[:, 0:512].rearrange("p (a b) -> p a b", a=4)), reads=[], writes=[ptk, k("TM")])
                for c in range(2):
                    P.op("pool", lambda e, c=c: e.tensor_scalar(out=c_["TMV%d" % c][:], in0=c_["TM"][:, 3, :], scalar1=hm[c], scalar2=None, op0=ALU.mult),
                         reads=[k("TM"), "msk"], writes=[k("TMV%d" % c)])

            def unit_chain(u, pc, dr, hd, last_of_pair):
                c_ = PC[pc]; n_ = UN[u]
                hs = slice(hd * 64, hd * 64 + 64)
                os_ = slice((1 - hd) * 64, (1 - hd) * 64 + 64)
                k = lambda n: (n, pc)
                uk = lambda n: (n, u)
                nmb, nmo = nm_reg(u); xmb, xmo = xm_reg(u)
                NMp = nmb[:, nmo:nmo + 256]; XMp = xmb[:, xmo:xmo + 128]
                nmk = xmk = ("PB", u)
                mk_s = 0 if dr == 0 else 1
                mk_i = 2 if dr == 0 else 3
                atm, btm, ktm, rt, TM = c_["atm%d" % hd], c_["btm%d" % hd], c_["ktm%d" % hd], c_["rt"], c_["TM"]
                ak, bk_, kk_ = k("atm%d" % hd), k("btm%d" % hd), k("ktm%d" % hd)
                P.op("pe", lambda e: e.matmul(NMp[:, 0:128], lhsT=btm[:], rhs=atm[:], start=True, stop=True), reads=[bk_, ak], writes=[nmk])
                P.op("pe", lambda e: e.matmul(NMp[:, 128:256], lhsT=atm[:], rhs=btm[:], start=True, stop=True), reads=[bk_, ak], writes=[nmk])
                P.op("pe", lambda e: e.matmul(XMp[:, 0:128], lhsT=ktm[:], rhs=atm[:], start=True, stop=True), reads=[kk_, ak], writes=[xmk])
                yield
                P.op("dve", lambda e: e.tensor_tensor(out=n_["NM0"][:], in0=NMp, in1=msk[:, mk_s, :], op=ALU.mult), reads=["msk"], writes=[nmk, uk("NM0")])
                P.op("dve", lambda e: e.tensor_tensor(out=n_["Ak"][:], in0=XMp, in1=msk[:, mk_s, 0:128], op=ALU.mult), reads=["msk"], writes=[xmk, uk("Ak")])
                yield
                P.op("pe", lambda e: e.matmul(XMp[:, 0:64], lhsT=n_["Ak"][:], rhs=TM[:, 3, hs], start=True, stop=True), reads=[uk("Ak"), k("TM")], writes=[xmk])
                P.op("pool", lambda e: e.tensor_copy(out=n_["X0"][:, hs], in_=TM[:, 0, hs]), reads=[k("TM")], writes=[uk("X0a")])
                P.op("pool", lambda e: e.tensor_copy(out=n_["Vpad"][:, os_], in_=TM[:, 3, hs]), reads=[k("TM")], writes=[("Vpad", u)])
                yield
                P.op("act", lambda e: e.copy(out=n_["X0"][:, os_], in_=XMp[:, 0:64]), reads=[], writes=[xmk, uk("X0b")])
                yield
                xkeys = [uk("X0a"), uk("X0b")]
                for j in range(6):
                    Xc = n_["X%d" % (j % 2)]; Xn = n_["X%d" % ((j + 1) % 2)]
                    NMc = n_["NM%d" % (j % 2)]; NMn = n_["NM%d" % ((j + 1) % 2)]
                    nck = uk("NM%d" % (j % 2)); nnk = uk("NM%d" % ((j + 1) % 2))
                    P.op("pe", lambda e, Xc=Xc, NMc=NMc: e.matmul(XMp[:, 0:128], lhsT=NMc[:, 0:128], rhs=Xc[:], start=True, stop=True), reads=[nck] + xkeys, writes=[xmk])
                    if j < 5:
                        P.op("pe", lambda e, NMc=NMc: e.matmul(NMp[:, 0:128], lhsT=NMc[:, 128:256], rhs=NMc[:, 0:128], start=True, stop=True), reads=[nck], writes=[nmk])
                        P.op("pe", lambda e, NMc=NMc: e.matmul(NMp[:, 128:256], lhsT=NMc[:, 0:128], rhs=NMc[:, 128:256], start=True, stop=True), reads=[nck], writes=[nmk])
                    yield
                    if (j + 1) % 2 == 1:
                        nxk = [uk("X1")]
                    else:
                        nxk = [uk("X0a"), uk("X0b")]
                    P.op("dve", lambda e, Xc=Xc, Xn=Xn: e.tensor_tensor(out=Xn[:], in0=XMp, in1=Xc[:], op=ALU.add), reads=xkeys, writes=[xmk] + nxk)
                    xkeys = nxk
                    if j < 5:
                        P.op("act", lambda e, NMn=NMn: e.copy(out=NMn[:], in_=NMp), reads=[], writes=[nmk, nnk])
                    yield
                X6 = n_["X0"]
                P.op("pe", lambda e: e.matmul(NMp[:, 0:128], lhsT=btm[:], rhs=rt[:], start=True, stop=True), reads=[bk_, k("rt")], writes=[nmk])
                P.op("pe", lambda e: e.matmul(NMp[:, 128:256], lhsT=ktm[:], rhs=rt[:], start=True, stop=True), reads=[kk_, k("rt")], writes=[nmk])
                for c in range(2):
                    P.op("pool", lambda e, c=c: e.tensor_scalar(out=n_["Xc%d" % c][:], in0=X6[:], scalar1=hm[c], scalar2=None, op0=ALU.mult),
                         reads=xkeys + ["msk"], writes=[uk("Xc%d" % c)])
                yield
                P.op("dve", lambda e: e.tensor_tensor(out=n_["Mr"][:], in0=NMp, in1=msk[:, mk_i, :], op=ALU.mult), reads=["msk"], writes=[nmk, uk("Mr")])
                yield
                P.op("pe", lambda e: e.matmul(XMp[:, 0:128], lhsT=X6[:], rhs=n_["Mr"][:, 0:128], start=True, stop=False), reads=xkeys + [uk("Mr")], writes=[xmk])
                P.op("pe", lambda e: e.matmul(XMp[:, 0:128], lhsT=n_["Vpad"][:], rhs=n_["Mr"][:, 128:256], start=False, stop=False), reads=[("Vpad", u), uk("Mr")], writes=[xmk])
                P.op("pe", lambda e: e.matmul(XMp[:, 0:128], lhsT=Ihs[hd][:], rhs=rt[:], start=False, stop=True), reads=[("Ihs", hd), k("rt")], writes=[xmk])
                for c in range(2):
                    P.op("pe", lambda e, c=c: e.matmul(NMp[:, c * 64:(c + 1) * 64], lhsT=n_["Xc%d" % c][:], rhs=TM[:, 1, hs], start=True, stop=True),
                         reads=[uk("Xc%d" % c), k("TM")], writes=[nmk])
                for c in range(2):
                    P.op("pe", lambda e, c=c: e.matmul(NMp[:, 128 + c * 64:128 + (c + 1) * 64], lhsT=TM[:, 1, :], rhs=n_["Xc%d" % c][:, os_], start=True, stop=False),
                         reads=[uk("Xc%d" % c), k("TM")], writes=[nmk])
                    P.op("pe", lambda e, c=c: e.matmul(NMp[:, 128 + c * 64:128 + (c + 1) * 64], lhsT=TM[:, 2, :], rhs=c_["TMV%d" % c][:, hs], start=False, stop=True),
                         reads=[k("TM"), k("TMV%d" % c)], writes=[nmk])
                yield
                P.op("act", lambda e: e.copy(out=n_["RpT"][:], in_=XMp[:, 0:128]), reads=[], writes=[xmk, uk("RpT")])
                P.op("dve", lambda e: e.tensor_copy(out=n_["OpT"][os_, :], in_=XMp[os_, 0:128]), reads=[], writes=[xmk, uk("OpT")])
                for c in range(2):
                    P.op("dve", lambda e, c=c: e.scalar_tensor_tensor(out=n_["GT"][hs, c, hs], in0=identf[hs, hs], scalar=c_["gC"][hs, c:c + 1],
                                                                      in1=NMp[hs, c * 64:(c + 1) * 64], op0=ALU.mult, op1=ALU.add),
                         reads=["identf", k("gC")], writes=[nmk, ("GT", u)])
                P.op("act", lambda e: e.copy(out=n_["J"][hs].rearrange("p c v -> p (c v)"), in_=NMp[hs, 128:256]), reads=[], writes=[nmk, uk("J")])
                yield
                order = (0, 1) if dr == 0 else (1, 0)
                for c in order:
                    P.op("pe", lambda e, c=c: e.matmul(XMp[:, 0:64], lhsT=n_["H"][:], rhs=n_["RpT"][:, c * 64:(c + 1) * 64], start=True, stop=True),
                         reads=[("H", u), uk("RpT")], writes=[xmk])
                    P.op("pe", lambda e, c=c: e.matmul(XMp[:, 64:128], lhsT=n_["GT"][:, c, :], rhs=n_["H"][:, os_], start=True, stop=True),
                         reads=[("H", u), ("GT", u)], writes=[xmk])
                    yield
                    P.op("dve", lambda e, c=c: e.tensor_tensor(out=c_["Oo"][os_, c * 64:(c + 1) * 64], in0=XMp[os_, 0:64], in1=n_["OpT"][os_, c * 64:(c + 1) * 64], op=ALU.add),
                         reads=[uk("OpT")], writes=[xmk, ("Oo", pc, hd)])
                    P.op("dve", lambda e, c=c: e.tensor_tensor(out=n_["H"][hs, os_], in0=XMp[hs, 64:128], in1=n_["J"][hs, c, :], op=ALU.add),
                         reads=[uk("J")], writes=[xmk, ("H", u)])
                    yield

            for i in range(per_b):
                gens = []
                infos = []
                for bt in range(nbatch):
                    for dr in range(2):
                        pc = bt * 2 + dr
                        blk = i if dr == 0 else per_b - 1 - i
                        pair_prep(pc, bt, dr, blk)
                        for hd in range(2):
                            u = pc * 2 + hd
                            gens.append(unit_chain(u, pc, dr, hd, hd == 1))
                        infos.append((pc, bt, dr, blk))
                import os
                CUT = int(os.environ.get("CUT", "1000"))
                nst = 0
                while gens and nst < CUT:
                    nst += 1
                    for g_ in list(gens):
                        try:
                            next(g_)
                        except StopIteration:
                            gens.remove(g_)
                for (pc, bt, dr, blk) in infos:
                    tok0 = bt * S + blk * 128
                    arr = A_OF if dr == 0 else A_OB
                    P.dma("sp", lambda e, pc=pc, arr=arr, tok0=tok0: e.dma_start(out=SC[arr, 0:64, tok0:tok0 + 128], in_=PC[pc]["Oo"][64:128, :]), ("oo", pc),
                          reads=[("Oo", pc, 0), ("Oo", pc, 1)], writes=[("SCO", arr, tok0 // 128)])
                    P.dma("sp", lambda e, pc=pc, arr=arr, tok0=tok0: e.dma_start(out=SC[arr, 64:128, tok0:tok0 + 128], in_=PC[pc]["Oo"][0:64, :]), ("oo", pc),
                          reads=[("Oo", pc, 0), ("Oo", pc, 1)], writes=[("SCO", arr, tok0 // 128)])
            P.flush()
        import os
        if "CUT" in os.environ:
            return

        with contextlib.ExitStack() as st:
            ld = [sbt(st, "a4ld%d" % i, [128, 4, 512], F32) for i in range(2)]
            o_ = sbt(st, "a4o", [128, 512], F32)
            cen = sbt(st, "a4cen", [128, 512], F32)
            sq = sbt(st, "a4sq", [128, 512], F32)
            rsd = sbt(st, "a4rsd", [128, 512], F32)
            yo = [sbt(st, "a4y%d" % i, [128, 512], F32) for i in range(2)]
            epsG = sbt(st, "epsG2", [128, 1], F32)
            pm = pst(st, "a4pm", [128, 512], F32)
            pvv = pst(st, "a4pv", [128, 512], F32)
            P.op("pool", lambda e: e.memset(epsG[:], GN_EPS), writes=["epsG"])
            for m in range(nblk):
                l_ = ld[m % 2]; lk = "a4ld%d" % (m % 2)
                for j, arr in enumerate((A_OF, A_OB, A_G, A_BON)):
                    rd = [("SCO", arr, m * 4 + q) for q in range(4)] if arr in (A_OF, A_OB) else [("SC", arr, m)]
                    P.dma("sp", lambda e, l_=l_, j=j, arr=arr, m=m: e.dma_start(out=l_[:, j, :], in_=SC[arr, :, m * 512:(m + 1) * 512]), (lk, j), reads=rd, writes=[(lk, j)])
                P.op("pool", lambda e, l_=l_: e.tensor_tensor(out=o_[:], in0=l_[:, 0, :], in1=l_[:, 1, :], op=ALU.add), reads=[(lk, 0), (lk, 1)], writes=["a4o"])
                P.op("pe", lambda e: e.matmul(pm[:], lhsT=BOf[:], rhs=o_[:], start=True, stop=True), reads=["BOf", "a4o"], writes=["a4pm"])
                P.op("dve", lambda e: e.scalar_tensor_tensor(out=cen[:], in0=pm[:], scalar=-1.0 / 64, in1=o_[:], op0=ALU.mult, op1=ALU.add), reads=["a4pm", "a4o"], writes=["a4cen"])
                P.op("pool", lambda e: e.tensor_tensor(out=sq[:], in0=cen[:], in1=cen[:], op=ALU.mult), reads=["a4cen"], writes=["a4sq"])
                P.op("pe", lambda e: e.matmul(pvv[:], lhsT=BOf[:], rhs=sq[:], start=True, stop=True), reads=["BOf", "a4sq"], writes=["a4pv"])
                P.op("act", lambda e: e.activation(out=rsd[:], in_=pvv[:], func=AF.Sqrt, bias=epsG[:, 0:1], scale=1.0 / 64), reads=["a4pv", "epsG"], writes=["a4rsd"])
                P.op("dve", lambda e: e.reciprocal(out=rsd[:], in_=rsd[:]), reads=["a4rsd"], writes=["a4rsd"])
                P.op("dve", lambda e: e.tensor_tensor(out=cen[:], in0=cen[:], in1=rsd[:], op=ALU.mult), reads=["a4cen", "a4rsd"], writes=["a4cen"])
                P.op("dve", lambda e: e.tensor_scalar(out=cen[:], in0=cen[:], scalar1=chs[:, 7:8], scalar2=chs[:, 8:9], op0=ALU.mult, op1=ALU.add), reads=["a4cen", "chs"], writes=["a4cen"])
                P.op("pool", lambda e, l_=l_: e.tensor_tensor(out=cen[:], in0=cen[:], in1=l_[:, 3, :], op=ALU.add), reads=["a4cen", (lk, 3)], writes=["a4cen"])
                y_ = yo[m % 2]; yk = "a4y%d" % (m % 2)
                P.op("pool", lambda e, l_=l_, y_=y_: e.tensor_tensor(out=y_[:], in0=cen[:], in1=l_[:, 2, :], op=ALU.mult), reads=["a4cen", (lk, 2)], writes=[yk])
                P.dma("sp", lambda e, y_=y_, m=m: e.dma_start(out=obT[:, m * 512:(m + 1) * 512], in_=y_[:]), ("yst", m % 2), reads=[yk], writes=[("obT", m)])
            P.flush()
    return


import contextlib
import numpy as np

D = 2048
GW = 64
NTOK = 2048
NBUF = 2560
NA_OFF = 0
RW_OFF = 3072
GATE_OFF = 3072 + 3712
EPS = 1e-6
NEG = -30000.0


def build_B(nc, stages="all", dbg=False):
    dt = nc.dram_tensor
    xh = dt("xh", [NBUF, D], F32, kind="ExternalInput").ap()
    cb = dt("cb", [D], F32, kind="ExternalInput").ap()
    ada_w = dt("ada_w", [D, 6 * D], F32, kind="ExternalInput").ap()
    ada_b = dt("ada_b", [6 * D], F32, kind="ExternalInput").ap()
    n1w = dt("norm1_w", [D], F32, kind="ExternalInput").ap()
    n2w = dt("norm2_w", [D], F32, kind="ExternalInput").ap()
    w_in = dt("w_in", [D, 10880], F32, kind="ExternalInput").ap()
    qnw = dt("qnw", [128], F32, kind="ExternalInput").ap()
    knw = dt("knw", [128], F32, kind="ExternalInput").ap()
    biasT = dt("biasT", [8, 128, 5, 512], F32, kind="ExternalInput").ap()
    if dbg:
        d_oa = dt("d_oa", [1024, NTOK], F32, kind="ExternalOutput").ap()
        d_mod = dt("d_mod", [128, 6, 16], F32, kind="ExternalOutput").ap()
        d_qn = dt("d_qn", [128, NTOK], BF16, kind="ExternalOutput").ap()
        d_kn = dt("d_kn", [128, NBUF], BF16, kind="ExternalOutput").ap()
        d_v0 = dt("d_v0", [128, 20, 128], BF16, kind="ExternalOutput").ap()
        d_S = dt("d_S", [128, 512], F32, kind="ExternalOutput").ap()
        d_E = dt("d_E", [128, 512], BF16, kind="ExternalOutput").ap()
        d_uT = dt("d_uT", [128, 16, NBUF], BF16, kind="ExternalOutput").ap()
    if stages != "na":
        b_gate = dt("b_gate", [2 * D], F32, kind="ExternalInput").ap()
        obT_in = dt("obT", [1024, NTOK], F32, kind="ExternalInput").ap()
        w_a = dt("w_branch_a", [1024, D], F32, kind="ExternalInput").ap()
        w_b = dt("w_branch_b", [1024, D], F32, kind="ExternalInput").ap()
        w_out = dt("w_out", [D, D], F32, kind="ExternalInput").ap()
        y = dt("y", [NTOK, D], F32, kind="ExternalOutput").ap()
        u2s = dt("u2s", [128, 16, NTOK], BF16, kind="Internal").ap()
    if stages == "all":
        w_q = dt("peer_w_q", [D, D], F32, kind="ExternalInput").ap()
        sk1 = dt("peer_sub_k1", [128, 128], F32, kind="ExternalInput").ap()
        sk2 = dt("peer_sub_k2", [128, 128], F32, kind="ExternalInput").ap()
        pu = dt("peer_u", [16384, D], F32, kind="ExternalInput").ap()
        pv = dt("peer_v", [16384, D], F32, kind="ExternalInput").ap()
        uts = dt("uts", [D, 16384], BF16, kind="Internal").ap()

    with contextlib.ExitStack() as top:
        P = Prog(nc, top)

        uid = [0]

        def sbt(st, name, shape, dtp):
            uid[0] += 1
            return st.enter_context(nc.sbuf_tensor("%s_u%d" % (name, uid[0]), shape, dtp))

        def pst(st, name, shape, dtp):
            uid[0] += 1
            return st.enter_context(nc.psum_tensor("%s_u%d" % (name, uid[0]), shape, dtp))

        ident = sbt(top, "ident", [128, 128], BF16)
        identf = sbt(top, "identf", [128, 128], F32)
        ones = sbt(top, "ones", [128, 128], BF16)
        modF = sbt(top, "modF", [128, 6, 16], F32)
        m1F = sbt(top, "m1F", [128, 16], F32)
        m2F = sbt(top, "m2F", [128, 16], F32)
        g1bc = sbt(top, "g1bc", [128, D], F32)
        g2bc = sbt(top, "g2bc", [128, D], F32)
        epsT = sbt(top, "epsT", [128, 1], F32)
        big_cm = contextlib.ExitStack()
        BIG = sbt(big_cm, "BIG", [128, 16, NBUF], BF16)

        P.op("pool", lambda e: e.memset(identf[:], 0.0), writes=["identf"])
        P.op("pool", lambda e: e.affine_select(out=identf[:], in_=identf[:], pattern=[[-1, 128]],
                                               compare_op=ALU.not_equal, fill=1.0, base=0, channel_multiplier=1),
             reads=["identf"], writes=["identf"])
        P.op("dve", lambda e: e.tensor_copy(out=ident[:], in_=identf[:]), reads=["identf"], writes=["ident"])
        P.op("pool", lambda e: e.memset(ones[:], 1.0), writes=["ones"])
        P.op("pool", lambda e: e.memset(epsT[:], EPS), writes=["epsT"])

        with contextlib.ExitStack() as st:
            cT = sbt(st, "cT", [128, 16], F32)
            scT = sbt(st, "scT", [128, 16], BF16)
            scB = sbt(st, "scB", [128, 16, 128], BF16)
            abF = sbt(st, "abF", [128, 6, 16], F32)
            nwF = sbt(st, "nwF", [128, 2, 16], F32)
            awb = [sbt(st, "awb%d" % i, [128, 16, 512], BF16) for i in range(2)]
            abbc = sbt(st, "abbc", [128, D], F32)
            pmod = pst(st, "pmod", [128, 512], F32)
            pg = pst(st, "pg", [128, 512], F32)
            P.dma("sp", lambda e: e.dma_start(out=cT[:], in_=cb.rearrange("(c p) -> p c", p=128), allow_slow_non_contiguous=True), "cT", writes=["cT"])
            P.dma("sp", lambda e: e.dma_start(out=abF[:], in_=ada_b.rearrange("(s c p) -> p s c", p=128, c=16), allow_slow_non_contiguous=True), "abF", writes=["abF"])
            P.dma("sp", lambda e: e.dma_start(out=nwF[:, 0, :], in_=n1w.rearrange("(c p) -> p c", p=128), allow_slow_non_contiguous=True), "nwF", writes=["nwF0"])
            P.dma("sp", lambda e: e.dma_start(out=nwF[:, 1, :], in_=n2w.rearrange("(c p) -> p c", p=128), allow_slow_non_contiguous=True), "nwF", writes=["nwF1"])
            P.op("act", lambda e: e.activation(out=scT[:], in_=cT[:], func=AF.Silu), reads=["cT"], writes=["scT"])
            P.op("dve", lambda e: e.tensor_copy(out=scB[:], in_=scT[:, :, None].to_broadcast([128, 16, 128])),
                 reads=["scT"], writes=["scB"])
            awr = ada_w.rearrange("(kc p) n -> p kc n", p=128)
            blk = 0
            for seg in range(6):
                for nb in range(4):
                    buf = awb[blk % 2]
                    bk = "awb%d" % (blk % 2)
                    col0 = seg * D + nb * 512
                    P.dma("pool", lambda e, buf=buf, col0=col0: e.dma_start(out=buf[:], in_=awr[:, :, col0:col0 + 512]),
                          bk, writes=[bk])
                    if seg in (2, 5):
                        for kc in range(16):
                            P.op("pe", lambda e, buf=buf, kc=kc: e.matmul(pg[:], lhsT=scB[:, kc, :], rhs=buf[:, kc, :],
                                                                          start=(kc == 0), stop=(kc == 15)),
                                 reads=[bk, "scB"], writes=["pg"])
                        if nb == 0:
                            P.dma("sp", lambda e, seg=seg: e.dma_start(out=abbc[:], in_=ada_b[seg * D:(seg + 1) * D].partition_broadcast(128)),
                                  "abbc", writes=["abbc"])
                        tgt = g1bc if seg == 2 else g2bc
                        tk = "g1bc" if seg == 2 else "g2bc"
                        P.op("dve", lambda e, tgt=tgt, nb=nb: e.tensor_tensor(out=tgt[:, nb * 512:(nb + 1) * 512], in0=pg[:],
                                                                              in1=abbc[:, nb * 512:(nb + 1) * 512], op=ALU.add),
                             reads=["pg", "abbc"], writes=[(tk, nb)])
                    else:
                        for j in range(4):
                            n = nb * 4 + j
                            for kc in range(16):
                                P.op("pe", lambda e, buf=buf, kc=kc, j=j, n=n: e.matmul(pmod[:, n:n + 1], lhsT=buf[:, kc, j * 128:(j + 1) * 128],
                                                                                       rhs=scT[:, kc:kc + 1], start=(kc == 0), stop=(kc == 15)),
                                     reads=[bk, "scT"], writes=["pmod"])
                        if nb == 3:
                            P.op("dve", lambda e, seg=seg: e.tensor_tensor(out=modF[:, seg, :], in0=pmod[:, 0:16], in1=abF[:, seg, :], op=ALU.add),
                                 reads=["pmod", "abF"], writes=[("modF", seg)])
                    blk += 1
            P.op("dve", lambda e: e.scalar_tensor_tensor(out=m1F[:], in0=modF[:, 1, :], scalar=1.0, in1=nwF[:, 0, :], op0=ALU.add, op1=ALU.mult),
                 reads=[("modF", 1), "nwF0"], writes=["m1F"])
            P.op("dve", lambda e: e.scalar_tensor_tensor(out=m2F[:], in0=modF[:, 4, :], scalar=1.0, in1=nwF[:, 1, :], op0=ALU.add, op1=ALU.mult),
                 reads=[("modF", 4), "nwF1"], writes=["m2F"])
            if dbg:
                P.dma("sp", lambda e: e.dma_start(out=d_mod[:, :, :], in_=modF[:]), "dbg", reads=[("modF", s) for s in (0, 1, 3, 4)])
            P.flush()

        def norm_tile(st_tiles, xt_key, xt, mF, shF_seg, pos, dstkey, ptr, ptrk):
            ss, rstd, xn, junk = st_tiles
            P.op("act", lambda e: e.activation(out=junk[:], in_=xt[:], func=AF.Square, accum_out=ss[:]),
                 reads=list(xt_key), writes=["junk", "ss"])
            P.op("dve", lambda e: e.tensor_scalar(out=rstd[:], in0=ss[:], scalar1=1.0 / D, scalar2=EPS, op0=ALU.mult, op1=ALU.add),
                 reads=["ss"], writes=["rstd"])
            P.op("dve", lambda e: e.reciprocal(out=rstd[:], in_=rstd[:]), reads=["rstd"], writes=["rstd"])
            P.op("act", lambda e: e.activation(out=rstd[:], in_=rstd[:], func=AF.Sqrt), reads=["rstd"], writes=["rstd"])
            P.op("dve", lambda e: e.tensor_scalar(out=xn[:], in0=xt[:], scalar1=rstd[:, 0:1], scalar2=None, op0=ALU.mult),
                 reads=list(xt_key) + ["rstd"], writes=["xn"])
            for g4 in range(4):
                for j in range(4):
                    c = g4 * 4 + j
                    P.op("pe", lambda e, c=c, j=j: e.transpose(out=ptr[:, j * 128:(j + 1) * 128], in_=xn[:, c * 128:(c + 1) * 128], identity=ident[:]),
                         reads=["xn", "ident"], writes=[ptrk])
                tmpk = "nt_tmp"
                P.op("dve", lambda e, g4=g4: e.tensor_tensor(out=nt_tmp[:].rearrange("p (a b) -> p a b", a=4),
                                                            in0=ptr[:, 0:512].rearrange("p (a b) -> p a b", a=4),
                                                            in1=mF[:, g4 * 4:(g4 + 1) * 4, None].to_broadcast([128, 4, 128]), op=ALU.mult),
                     reads=[ptrk, "m1F", "m2F"], writes=[tmpk])
                P.op("pool", lambda e, g4=g4: e.tensor_tensor(out=BIG[:, g4 * 4:(g4 + 1) * 4, pos:pos + 128],
                                                             in0=nt_tmp[:].rearrange("p (a b) -> p a b", a=4),
                                                             in1=modF[:, shF_seg, g4 * 4:(g4 + 1) * 4, None].to_broadcast([128, 4, 128]), op=ALU.add),
                     reads=[tmpk, ("modF", shF_seg)], writes=[dstkey])

        with contextlib.ExitStack() as st:
            xts = [sbt(st, "xt%d" % i, [128, D], F32) for i in range(2)]
            ss = sbt(st, "ss", [128, 1], F32)
            rstd = sbt(st, "rstd", [128, 1], F32)
            xn = sbt(st, "xn", [128, D], BF16)
            junk = sbt(st, "junk", [128, D], F32)
            nt_tmp = sbt(st, "nt_tmp", [128, 512], F32)
            ptr = pst(st, "ptr", [128, 1024], BF16)
            for t in range(20):
                xt = xts[t % 2]
                xk = "xt%d" % (t % 2)
                P.dma("sp", lambda e, xt=xt, t=t: e.dma_start(out=xt[:], in_=xh[t * 128:(t + 1) * 128, :]), xk, writes=[xk])
                norm_tile((ss, rstd, xn, junk), [xk], xt, m1F, 0, t * 128, ("BIG", t), ptr, "ptr")
            P.flush()

        oaT_cm = contextlib.ExitStack()
        oaT = sbt(oaT_cm, "oaT", [128, 8, NTOK], BF16)
        with contextlib.ExitStack() as st:
            wq_t = [sbt(st, "wq%d" % i, [128, 16, 128], BF16) for i in range(2)]
            wk_t = [sbt(st, "wk%d" % i, [128, 16, 128], BF16) for i in range(2)]
            wv_t = [sbt(st, "wv%d" % i, [128, 16, 128], BF16) for i in range(2)]
            bt_t = [sbt(st, "bt%d" % i, [128, 5, 512], F32) for i in range(2)]
            qn = sbt(st, "qn", [128, NTOK], BF16)
            kn = sbt(st, "kn", [128, NBUF], BF16)
            V0 = sbt(st, "V0", [128, 20, 128], BF16)
            V1 = sbt(st, "V1", [128, 19, 128], BF16)
            sq = sbt(st, "sq", [128, 512], BF16)
            rt = sbt(st, "rt", [128, 512], F32)
            nwq = sbt(st, "nwq", [128, 2], F32)
            S_t = [sbt(st, "S%d" % i, [128, 512], F32) for i in range(2)]
            E_t = [sbt(st, "E%d" % i, [128, 512], BF16) for i in range(2)]
            rs_t = [sbt(st, "rs%d" % i, [128, 128], F32) for i in range(2)]
            pq = [pst(st, "pq%d" % i, [128, 512], F32) for i in range(2)]
            pss = pst(st, "pss", [128, 512], F32)
            pL = [pst(st, "pL%d" % i, [128, 512], F32) for i in range(2)]
            pO = [pst(st, "pO%d" % i, [128, 512], F32) for i in range(2)]
            P.dma("sp", lambda e: e.dma_start(out=nwq[:, 0:1], in_=qnw.rearrange("(p o) -> p o", o=1), allow_slow_non_contiguous=True), "nwq", writes=["nwq0"])
            P.dma("sp", lambda e: e.dma_start(out=nwq[:, 1:2], in_=knw.rearrange("(p o) -> p o", o=1), allow_slow_non_contiguous=True), "nwq", writes=["nwq1"])
            P.op("dve", lambda e: e.tensor_scalar(out=nwq[:, 0:1], in0=nwq[:, 0:1], scalar1=128.0 ** -0.5, scalar2=None, op0=ALU.mult),
                 reads=["nwq0"], writes=["nwq0"])
            winr = w_in.rearrange("(kc p) n -> p kc n", p=128)
            cnt_blk = 0
            for h in range(8):
                hb = h % 2
                for (wt, off, nm) in ((wq_t, 0, "wq"), (wk_t, 1024, "wk"), (wv_t, 2048, "wv")):
                    P.dma("pool", lambda e, wt=wt, off=off, h=h, hb=hb: e.dma_start(out=wt[hb][:], in_=winr[:, :, off + h * 128: off + (h + 1) * 128]),
                          "%s%d" % (nm, hb), writes=["%s%d" % (nm, hb)])
                P.dma("sp", lambda e, h=h, hb=hb: e.dma_start(out=bt_t[hb][:], in_=biasT[h]), "bt%d" % hb, writes=["bt%d" % hb])
                for (which, wt, nblk, tok0, dst, dk, wcol) in (("q", wq_t, 4, 256, qn, "qn", 0), ("k", wk_t, 5, 0, kn, "kn", 1)):
                    for bi in range(nblk):
                        pp = pq[cnt_blk % 2]
                        pk = "pq%d" % (cnt_blk % 2)
                        cnt_blk += 1
                        t0 = tok0 + bi * 512
                        for kc in range(16):
                            P.op("pe", lambda e, pp=pp, wt=wt, kc=kc, t0=t0, hb=hb: e.matmul(pp[:], lhsT=wt[hb][:, kc, :], rhs=BIG[:, kc, t0:t0 + 512],
                                                                                         start=(kc == 0), stop=(kc == 15)),
                                 reads=["%s%d" % ("wq" if which == "q" else "wk", hb)] + [("BIG", tt) for tt in range(t0 // 128, t0 // 128 + 4)],
                                 writes=[pk])
                        P.op("act", lambda e, pp=pp: e.activation(out=sq[:], in_=pp[:], func=AF.Square), reads=[pk], writes=["sq"])
                        P.op("pe", lambda e: e.matmul(pss[:], lhsT=ones[:], rhs=sq[:], start=True, stop=True), reads=["sq", "ones"], writes=["pss"])
                        P.op("act", lambda e: e.activation(out=rt[:], in_=pss[:], func=AF.Sqrt, bias=epsT[:, 0:1], scale=1.0 / 128),
                             reads=["pss", "epsT"], writes=["rt"])
                        P.op("dve", lambda e: e.reciprocal(out=rt[:], in_=rt[:]), reads=["rt"], writes=["rt"])
                        P.op("dve", lambda e, pp=pp, dst=dst, bi=bi, wcol=wcol: e.scalar_tensor_tensor(
                            out=dst[:, bi * 512:(bi + 1) * 512], in0=pp[:], scalar=nwq[:, wcol:wcol + 1], in1=rt[:], op0=ALU.mult, op1=ALU.mult),
                            reads=[pk, "rt", "nwq0", "nwq1"], writes=[(dk, bi)])
                for (Vt, vk, ntile, toff) in ((V0, "V0", 20, 0), (V1, "V1", 19, 64)):
                    for g4 in range(0, ntile, 4):
                        pp = pq[cnt_blk % 2]
                        pk = "pq%d" % (cnt_blk % 2)
                        cnt_blk += 1
                        n4 = min(4, ntile - g4)
                        for j in range(n4):
                            t0 = toff + (g4 + j) * 128
                            for kc in range(16):
                                P.op("pe", lambda e, pp=pp, j=j, kc=kc, t0=t0, hb=hb: e.matmul(pp[:, j * 128:(j + 1) * 128], lhsT=BIG[:, kc, t0:t0 + 128],
                                                                                          rhs=wv_t[hb][:, kc, :], start=(kc == 0), stop=(kc == 15)),
                                     reads=["wv%d" % hb] + [("BIG", tt) for tt in range(t0 // 128, min(20, (t0 + 127) // 128 + 1))], writes=[pk])
                        P.op("act", lambda e, pp=pp, Vt=Vt, g4=g4, n4=n4: e.copy(out=Vt[:, g4:g4 + n4, :], in_=pp[:, 0:n4 * 128].rearrange("p (a b) -> p a b", a=n4)),
                             reads=[pk], writes=[(vk, g4)])
                for ip in range(16):
                    pl = pL[ip % 2]; plk = "pL%d" % (ip % 2)
                    po = pO[ip % 2]; pok = "pO%d" % (ip % 2)
                    S = S_t[ip % 2]; Sk = "S%d" % (ip % 2)
                    E = E_t[ip % 2]; Ek = "E%d" % (ip % 2)
                    rs = rs_t[ip % 2]; rsk = "rs%d" % (ip % 2)
                    slot = {0: 1, 1: 2, 14: 3, 15: 4}.get(ip, 0)
                    for r in range(2):
                        il = 2 * ip + r
                        for j in range(4):
                            k0 = il * 64 + j * 128
                            P.op("pe", lambda e, pl=pl, r=r, j=j, k0=k0, il=il: e.matmul(pl[:, (r * 4 + j) * 64:(r * 4 + j + 1) * 64], lhsT=kn[:, k0:k0 + 128],
                                                                                   rhs=qn[:, il * 64:(il + 1) * 64], start=True, stop=True),
                                 reads=[("kn", b) for b in range(k0 // 512, (k0 + 127) // 512 + 1)] + [("qn", il * 64 // 512)], writes=[plk])
                    P.op("dve", lambda e, pl=pl, S=S, slot=slot, hb=hb: e.tensor_tensor(out=S[:], in0=pl[:], in1=bt_t[hb][:, slot, :], op=ALU.add),
                         reads=[plk, "bt%d" % hb], writes=[Sk])
                    P.op("act", lambda e, S=S, E=E: e.activation(out=E[:], in_=S[:], func=AF.Exp), reads=[Sk], writes=[Ek])
                    for r in range(2):
                        il = 2 * ip + r
                        for j in range(4):
                            if il % 2 == 0:
                                Vt, vk, ti = V0, "V0", il // 2 + j
                            else:
                                Vt, vk, ti = V1, "V1", (il - 1) // 2 + j
                            P.op("pe", lambda e, po=po, r=r, j=j, Vt=Vt, ti=ti, E=E: e.matmul(po[:, (r * 2) * 64:(r * 2 + 1) * 64], lhsT=Vt[:, ti, :],
                                                                                         rhs=E[:, (r * 4 + j) * 64:(r * 4 + j + 1) * 64], start=(j == 0), stop=(j == 3)),
                                 reads=[(vk, (ti // 4) * 4), Ek], writes=[pok])
                        for j in range(4):
                            P.op("pe", lambda e, po=po, r=r, j=j, E=E: e.matmul(po[:, (r * 2 + 1) * 64:(r * 2 + 2) * 64], lhsT=ones[:],
                                                                           rhs=E[:, (r * 4 + j) * 64:(r * 4 + j + 1) * 64], start=(j == 0), stop=(j == 3)),
                                 reads=["ones", Ek], writes=[pok])
                    pov = po[:, 0:256].rearrange("p (r s q) -> p r s q", r=2, s=2)
                    P.op("dve", lambda e, pov=pov, rs=rs: e.reciprocal(out=rs[:].rearrange("p (r q) -> p r q", r=2), in_=pov[:, :, 1, :]),
                         reads=[pok], writes=[rsk])
                    P.op("dve", lambda e, pov=pov, rs=rs, h=h, ip=ip: e.tensor_tensor(out=oaT[:, h, ip * 128:(ip + 1) * 128].rearrange("p (r q) -> p r q", r=2),
                                                                                 in0=pov[:, :, 0, :], in1=rs[:].rearrange("p (r q) -> p r q", r=2), op=ALU.mult),
                         reads=[pok, rsk], writes=[("oaT", h)])
            if dbg:
                P.dma("sp", lambda e: e.dma_start(out=d_qn[:, :], in_=qn[:]), "dbg", reads=[("qn", i) for i in range(4)])
                P.dma("sp", lambda e: e.dma_start(out=d_kn[:, :], in_=kn[:]), "dbg", reads=[("kn", i) for i in range(5)])
                P.dma("sp", lambda e: e.dma_start(out=d_v0[:, :, :], in_=V0[:]), "dbg", reads=[("V0", i) for i in range(0, 20, 4)])
                P.dma("sp", lambda e: e.dma_start(out=d_S[:, :], in_=S_t[1][:]), "dbg", reads=["S1"])
                P.dma("sp", lambda e: e.dma_start(out=d_E[:, :], in_=E_t[1][:]), "dbg", reads=["E1"])
                P.dma("sp", lambda e: e.dma_start(out=d_uT[:, :, :], in_=BIG[:]), "dbg", reads=[("BIG", i) for i in range(20)])
                dbgf = sbt(st, "dbgf", [128, 8, 128], F32)
                for qd in range(16):
                    P.op("dve", lambda e, qd=qd: e.tensor_copy(out=dbgf[:], in_=oaT[:, :, qd * 128:(qd + 1) * 128]), reads=[("oaT", h) for h in range(8)], writes=["dbgf"])
                    P.dma("sp", lambda e, qd=qd: e.dma_start(out=d_oa.rearrange("(h p) t -> p h t", p=128)[:, :, qd * 128:(qd + 1) * 128], in_=dbgf[:]), "dbg", reads=["dbgf"])
            P.flush()
        if stages == "na":
            oaT_cm.close()
            return

        with contextlib.ExitStack() as st:
            wg_t = [[sbt(st, "wg%d_%d" % (ab, i), [128, 16, 256], BF16) for i in range(2)] for ab in range(2)]
            wab_t = [[sbt(st, "wab%d_%d" % (ab, i), [128, 8, 256], BF16) for i in range(2)] for ab in range(2)]
            ob_t = [sbt(st, "obt%d" % i, [128, 8, 256], BF16) for i in range(2)]
            bgF = sbt(st, "bgF", [128, 32], F32)
            sg_t = [[sbt(st, "sg%d_%d" % (ab, i), [128, 256], F32) for i in range(2)] for ab in range(2)]
            mm_t = [[sbt(st, "mm%d_%d" % (ab, i), [128, 256], F32) for i in range(2)] for ab in range(2)]
            pgs = [[pst(st, "pgs%d_%d" % (ab, i), [128, 512], F32) for i in range(2)] for ab in range(2)]
            pab = [[pst(st, "pab%d_%d" % (ab, i), [128, 512], F32) for i in range(2)] for ab in range(2)]
            P.dma("sp", lambda e: e.dma_start(out=bgF[:], in_=b_gate.rearrange("(c p) -> p c", p=128), allow_slow_non_contiguous=True), "bgF", writes=["bgF"])
            winr = w_in.rearrange("(kc p) n -> p kc n", p=128)
            war = [w_a.rearrange("(kc p) n -> p kc n", p=128), w_b.rearrange("(kc p) n -> p kc n", p=128)]
            obr = obT_in.rearrange("(kc p) t -> p kc t", p=128)
            it = 0
            for tb in range(8):
                ob = ob_t[tb % 2]; obk = "obt%d" % (tb % 2)
                P.dma("pool", lambda e, ob=ob, tb=tb: e.dma_start(out=ob[:], in_=obr[:, :, tb * 256:(tb + 1) * 256]), obk, writes=[obk])
                up0 = 256 + tb * 256
                ukeys = [("BIG", up0 // 128), ("BIG", up0 // 128 + 1)]
                for cg in range(8):
                    wb_i = (tb * 8 + cg) % 2
                    for ab in range(2):
                        col0 = GATE_OFF + ab * D + cg * 256
                        P.dma("pool", lambda e, ab=ab, wb_i=wb_i, col0=col0: e.dma_start(out=wg_t[ab][wb_i][:], in_=winr[:, :, col0:col0 + 256]),
                              "wg%d_%d" % (ab, wb_i), writes=["wg%d_%d" % (ab, wb_i)])
                        P.dma("pool", lambda e, ab=ab, wb_i=wb_i, cg=cg: e.dma_start(out=wab_t[ab][wb_i][:], in_=war[ab][:, :, cg * 256:(cg + 1) * 256]),
                              "wab%d_%d" % (ab, wb_i), writes=["wab%d_%d" % (ab, wb_i)])
                    for c2 in range(2):
                        cc = cg * 2 + c2
                        pi = it % 2
                        it += 1
                        for ab in range(2):
                            pg_ = pgs[ab][pi]; pgk = "pgs%d_%d" % (ab, pi)
                            for kc in range(16):
                                P.op("pe", lambda e, pg_=pg_, ab=ab, wb_i=wb_i, kc=kc, c2=c2, up0=up0: e.matmul(
                                    pg_[:, 0:256], lhsT=wg_t[ab][wb_i][:, kc, c2 * 128:(c2 + 1) * 128], rhs=BIG[:, kc, up0:up0 + 256],
                                    start=(kc == 0), stop=(kc == 15)), reads=["wg%d_%d" % (ab, wb_i)] + ukeys, writes=[pgk])
                            pa_ = pab[ab][pi]; pak = "pab%d_%d" % (ab, pi)
                            for kc in range(8):
                                if ab == 0:
                                    P.op("pe", lambda e, pa_=pa_, wb_i=wb_i, kc=kc, c2=c2, tb=tb: e.matmul(
                                        pa_[:, 0:256], lhsT=wab_t[0][wb_i][:, kc, c2 * 128:(c2 + 1) * 128], rhs=oaT[:, kc, tb * 256:(tb + 1) * 256],
                                        start=(kc == 0), stop=(kc == 7)), reads=["wab0_%d" % wb_i] + [("oaT", hh) for hh in range(8)], writes=[pak])
                                else:
                                    P.op("pe", lambda e, pa_=pa_, wb_i=wb_i, kc=kc, c2=c2, ob=ob: e.matmul(
                                        pa_[:, 0:256], lhsT=wab_t[1][wb_i][:, kc, c2 * 128:(c2 + 1) * 128], rhs=ob[:, kc, :],
                                        start=(kc == 0), stop=(kc == 7)), reads=["wab1_%d" % wb_i, obk], writes=[pak])
                            sg = sg_t[ab][pi]; sgk = "sg%d_%d" % (ab, pi)
                            P.op("act", lambda e, sg=sg, pg_=pg_, ab=ab, cc=cc: e.activation(out=sg[:], in_=pg_[:, 0:256], func=AF.Sigmoid,
                                                                                           bias=bgF[:, ab * 16 + cc: ab * 16 + cc + 1]),
                                 reads=[pgk, "bgF"], writes=[sgk])
                            mmt = mm_t[ab][pi]; mmk = "mm%d_%d" % (ab, pi)
                            P.op("dve", lambda e, mmt=mmt, sg=sg, pa_=pa_: e.tensor_tensor(out=mmt[:], in0=pa_[:, 0:256], in1=sg[:], op=ALU.mult),
                                 reads=[pak, sgk], writes=[mmk])
                        mp0 = tb * 256
                        P.op("pool", lambda e, pi=pi, cc=cc, mp0=mp0: e.tensor_tensor(out=BIG[:, cc, mp0:mp0 + 256], in0=mm_t[0][pi][:], in1=mm_t[1][pi][:], op=ALU.add),
                             reads=["mm0_%d" % pi, "mm1_%d" % pi], writes=[("BIG", mp0 // 128), ("BIG", mp0 // 128 + 1)])
            P.flush()
        oaT_cm.close()

        with contextlib.ExitStack() as st:
            wo_t = [sbt(st, "wo%d" % i, [128, 16, 512], BF16) for i in range(2)]
            xts = [sbt(st, "xt%d" % i, [128, D], F32) for i in range(2)]
            x1ts = [sbt(st, "x1t%d" % i, [128, D], F32) for i in range(2)]
            tmpw = [sbt(st, "tmpw%d" % i, [128, 512], F32) for i in range(2)]
            ss = sbt(st, "ss", [128, 1], F32)
            rstd = sbt(st, "rstd", [128, 1], F32)
            xn = sbt(st, "xn", [128, D], BF16)
            junk = sbt(st, "junk", [128, D], F32)
            nt_tmp = sbt(st, "nt_tmp", [128, 512], F32)
            ptr = pst(st, "ptr", [128, 1024], BF16)
            pwo = [pst(st, "pwo%d" % i, [128, 512], F32) for i in range(2)]
            wor = w_out.rearrange("(kc p) n -> p kc n", p=128)
            it = 0
            for tt in range(16):
                xt = xts[tt % 2]; xk = "xt%d" % (tt % 2)
                x1t = x1ts[tt % 2]; x1k = "x1t%d" % (tt % 2)
                P.dma("sp", lambda e, xt=xt, tt=tt: e.dma_start(out=xt[:], in_=xh[256 + tt * 128: 256 + (tt + 1) * 128, :]), xk, writes=[xk])
                for db in range(4):
                    wi = it % 2; it += 1
                    P.dma("pool", lambda e, wi=wi, db=db: e.dma_start(out=wo_t[wi][:], in_=wor[:, :, db * 512:(db + 1) * 512]), "wo%d" % wi, writes=["wo%d" % wi])
                    pw = pwo[wi]; pwk = "pwo%d" % wi
                    for kc in range(16):
                        P.op("pe", lambda e, pw=pw, wi=wi, kc=kc, tt=tt: e.matmul(pw[:], lhsT=BIG[:, kc, tt * 128:(tt + 1) * 128], rhs=wo_t[wi][:, kc, :],
                                                                                  start=(kc == 0), stop=(kc == 15)),
                             reads=["wo%d" % wi, ("BIG", tt)], writes=[pwk])
                    tw = tmpw[wi]; twk = "tmpw%d" % wi
                    P.op("dve", lambda e, tw=tw, pw=pw, db=db: e.tensor_tensor(out=tw[:], in0=pw[:], in1=g1bc[:, db * 512:(db + 1) * 512], op=ALU.mult),
                         reads=[pwk] + [("g1bc", db)], writes=[twk])
                    P.op("pool", lambda e, tw=tw, x1t=x1t, xt=xt, db=db: e.tensor_tensor(out=x1t[:, db * 512:(db + 1) * 512], in0=tw[:], in1=xt[:, db * 512:(db + 1) * 512], op=ALU.add),
                         reads=[twk, xk], writes=[(x1k, db)])
                P.dma("sp", lambda e, x1t=x1t, tt=tt: e.dma_start(out=y[tt * 128:(tt + 1) * 128, :], in_=x1t[:]), "ystore", reads=[(x1k, db) for db in range(4)], writes=[("y", tt)])
                norm_tile((ss, rstd, xn, junk), [(x1k, db) for db in range(4)], x1t, m2F, 3, tt * 128, ("BIG", tt), ptr, "ptr")
                P.dma("sp", lambda e, tt=tt: e.dma_start(out=u2s[:, :, tt * 128:(tt + 1) * 128], in_=BIG[:, :, tt * 128:(tt + 1) * 128]), "u2store",
                      reads=[("BIG", tt)], writes=[("u2s", tt)])
            P.flush()
        big_cm.close()
        if stages == "mix":
            return

        K1T = sbt(top, "K1T", [128, 128], BF16)
        K2T = sbt(top, "K2T", [128, 128], BF16)
        with contextlib.ExitStack() as st:
            skt = [sbt(st, "skt%d" % i, [128, 128], F32) for i in range(2)]
            pkt = pst(st, "pkt", [128, 512], F32)
            put = [sbt(st, "put%d" % i, [128, D], BF16) for i in range(2)]
            utt = [sbt(st, "utt%d" % i, [128, 16, 128], BF16) for i in range(2)]
            ptr2 = [pst(st, "ptr2_%d" % i, [128, 1024], BF16) for i in range(2)]
            for i, (skd, KT, kk) in enumerate(((sk1, K1T, "K1T"), (sk2, K2T, "K2T"))):
                P.dma("sp", lambda e, i=i, skd=skd: e.dma_start(out=skt[i][:], in_=skd[:, :]), "skt%d" % i, writes=["skt%d" % i])
                P.op("pe", lambda e, i=i: e.transpose(out=pkt[:, 0:128], in_=skt[i][:], identity=identf[:]), reads=["skt%d" % i, "identf"], writes=["pkt"])
                P.op("dve", lambda e, KT=KT: e.tensor_copy(out=KT[:], in_=pkt[:, 0:128]), reads=["pkt"], writes=[kk])
            utsr = uts.rearrange("(c p) e -> p c e", p=128)
            it = 0
            for et in range(128):
                pb = put[et % 2]; pbk = "put%d" % (et % 2)
                ub = utt[et % 2]; ubk = "utt%d" % (et % 2)
                P.dma("pool", lambda e, pb=pb, et=et: e.dma_start(out=pb[:], in_=pu[et * 128:(et + 1) * 128, :]), pbk, writes=[pbk])
                for c4 in range(4):
                    pt2 = ptr2[it % 2]; ptk = "ptr2_%d" % (it % 2); it += 1
                    for j in range(4):
                        c = c4 * 4 + j
                        P.op("pe", lambda e, pt2=pt2, pb=pb, c=c, j=j: e.transpose(out=pt2[:, j * 128:(j + 1) * 128], in_=pb[:, c * 128:(c + 1) * 128], identity=ident[:]),
                             reads=[pbk, "ident"], writes=[ptk])
                    eng = "act" if c4 % 2 == 0 else "dve"
                    if eng == "act":
                        P.op("act", lambda e, pt2=pt2, ub=ub, c4=c4: e.copy(out=ub[:, c4 * 4:(c4 + 1) * 4, :], in_=pt2[:, 0:512].rearrange("p (a b) -> p a b", a=4)),
                             reads=[ptk], writes=[(ubk, c4)])
                    else:
                        P.op("dve", lambda e, pt2=pt2, ub=ub, c4=c4: e.tensor_copy(out=ub[:, c4 * 4:(c4 + 1) * 4, :], in_=pt2[:, 0:512].rearrange("p (a b) -> p a b", a=4)),
                             reads=[ptk], writes=[(ubk, c4)])
                P.dma("sp", lambda e, ub=ub, et=et: e.dma_start(out=utsr[:, :, et * 128:(et + 1) * 128], in_=ub[:]), "utst%d" % (et % 2),
                      reads=[(ubk, c4) for c4 in range(4)], writes=[("uts", et)])
            P.flush()

        with contextlib.ExitStack() as stq:
            uq = sbt(stq, "uq", [128, 16, 512], BF16)
            s1pp = sbt(stq, "s1pp", [128, 4, 8, 128], F32)
            s2s = sbt(stq, "s2s", [128, 4, 8, 128], F32)
            lnw = sbt(stq, "lnw", [128, 4, 8], F32)
            UTg = sbt(stq, "UTg", [128, 16, 512], BF16)
            Vg = sbt(stq, "Vg", [128, 4, D], BF16)
            acc = sbt(stq, "acc", [128, 4, D], F32)
            wqr = w_q.rearrange("(kc p) n -> p kc n", p=128)
            for Q in range(4):
                P.dma("sp", lambda e, Q=Q: e.dma_start(out=uq[:], in_=u2s[:, :, Q * 512:(Q + 1) * 512]), "uq", writes=["uq"])
                with contextlib.ExitStack() as st:
                    qTq = sbt(st, "qTq", [128, 16, 512], BF16)
                    wqt = [sbt(st, "wqt%d" % i, [128, 16, 128], BF16) for i in range(2)]
                    s1r = sbt(st, "s1r", [128, 8, 128], F32)
                    tmpk = sbt(st, "tmpk", [128, 128], F32)
                    v1 = sbt(st, "v1", [128, 8, 16], F32)
                    v2 = sbt(st, "v2", [128, 8, 16], F32)
                    cand = sbt(st, "cand", [128, 8, 256], F32)
                    tmpc = sbt(st, "tmpc", [128, 256], F32)
                    ts = sbt(st, "ts", [128, 8, 16], F32)
                    e16 = sbt(st, "e16", [128, 8, 16], F32)
                    Zs = sbt(st, "Zs", [128, 8], F32)
                    tmm = sbt(st, "tmm", [128, 8], F32)
                    pqq = [pst(st, "pqq%d" % i, [128, 512], F32) for i in range(2)]
                    psc = [pst(st, "psc%d" % i, [128, 512], F32) for i in range(4)]
                    for jh in range(16):
                        wb_ = wqt[jh % 2]; wbk = "wqt%d" % (jh % 2)
                        P.dma("pool", lambda e, wb_=wb_, jh=jh: e.dma_start(out=wb_[:], in_=wqr[:, :, jh * 128:(jh + 1) * 128]), wbk, writes=[wbk])
                        pq_ = pqq[jh % 2]; pqk = "pqq%d" % (jh % 2)
                        for kc in range(16):
                            P.op("pe", lambda e, pq_=pq_, wb_=wb_, kc=kc: e.matmul(pq_[:], lhsT=wb_[:, kc, :], rhs=uq[:, kc, :], start=(kc == 0), stop=(kc == 15)),
                                 reads=[wbk, "uq"], writes=[pqk])
                        if jh % 2 == 0:
                            P.op("act", lambda e, pq_=pq_, jh=jh: e.copy(out=qTq[:, jh, :], in_=pq_[:]), reads=[pqk], writes=[("qTq", jh)])
                        else:
                            P.op("dve", lambda e, pq_=pq_, jh=jh: e.tensor_copy(out=qTq[:, jh, :], in_=pq_[:]), reads=[pqk], writes=[("qTq", jh)])
                    for tl in range(4):
                        for half in range(2):
                            KT = K1T if half == 0 else K2T
                            for h in range(8):
                                pb_ = psc[half * 2 + h // 4]; pbk_ = "psc%d" % (half * 2 + h // 4)
                                P.op("pe", lambda e, pb_=pb_, h=h, half=half, KT=KT, tl=tl: e.matmul(pb_[:, (h % 4) * 128:(h % 4 + 1) * 128],
                                                                                                   lhsT=qTq[:, 2 * h + half, tl * 128:(tl + 1) * 128], rhs=KT[:],
                                                                                                   start=True, stop=True),
                                     reads=[("qTq", 2 * h + half), "K1T", "K2T"], writes=[pbk_])
                        for hh in range(2):
                            P.op("act", lambda e, hh=hh: e.copy(out=s1r[:, hh * 4:(hh + 1) * 4, :], in_=psc[hh][:].rearrange("p (a b) -> p a b", a=4)),
                                 reads=["psc%d" % hh], writes=[("s1r", hh)])
                            P.op("act", lambda e, hh=hh, tl=tl: e.copy(out=s2s[:, tl, hh * 4:(hh + 1) * 4, :], in_=psc[2 + hh][:].rearrange("p (a b) -> p a b", a=4)),
                                 reads=["psc%d" % (2 + hh)], writes=[("s2s", tl, hh)])
                        for (src_fn, vv, vk, rk) in ((lambda h: s1r[:, h, :], v1, "v1", [("s1r", 0), ("s1r", 1)]),
                                                     (lambda h, tl=tl: s2s[:, tl, h, :], v2, "v2", [("s2s", tl, 0), ("s2s", tl, 1)])):
                            for h in range(8):
                                P.op("dve", lambda e, h=h, vv=vv, src_fn=src_fn: e.max(out=vv[:, h, 0:8], in_=src_fn(h)), reads=rk, writes=[(vk, h, 0)])
                                P.op("dve", lambda e, h=h, vv=vv, src_fn=src_fn: e.match_replace(out=tmpk[:], in_to_replace=vv[:, h, 0:8], in_values=src_fn(h), imm_value=-1e30),
                                     reads=rk + [(vk, h, 0)], writes=["tmpk"])
                                P.op("dve", lambda e, h=h, vv=vv: e.max(out=vv[:, h, 8:16], in_=tmpk[:]), reads=["tmpk"], writes=[(vk, h, 1)])
                        vkeys = [(k_, h, a) for k_ in ("v1", "v2") for h in range(8) for a in range(2)]
                        P.op("dve", lambda e: e.tensor_tensor(out=cand[:].rearrange("p h (a b) -> p h a b", a=16),
                                                              in0=v1[:, :, :, None].to_broadcast([128, 8, 16, 16]),
                                                              in1=v2[:, :, None, :].to_broadcast([128, 8, 16, 16]), op=ALU.add),
                             reads=vkeys, writes=["cand"])
                        for h in range(8):
                            P.op("dve", lambda e, h=h: e.max(out=ts[:, h, 0:8], in_=cand[:, h, :]), reads=["cand"], writes=[("ts", h, 0)])
                            P.op("dve", lambda e, h=h: e.match_replace(out=tmpc[:], in_to_replace=ts[:, h, 0:8], in_values=cand[:, h, :], imm_value=-1e30),
                                 reads=["cand", ("ts", h, 0)], writes=["tmpc"])
                            P.op("dve", lambda e, h=h: e.max(out=ts[:, h, 8:16], in_=tmpc[:]), reads=["tmpc"], writes=[("ts", h, 1)])
                        tkeys = [("ts", h, a) for h in range(8) for a in range(2)]
                        P.op("dve", lambda e: e.tensor_tensor(out=e16[:], in0=ts[:], in1=ts[:, :, 0:1].to_broadcast([128, 8, 16]), op=ALU.subtract),
                             reads=tkeys, writes=["e16"])
                        P.op("act", lambda e: e.activation(out=e16[:], in_=e16[:], func=AF.Exp), reads=["e16"], writes=["e16"])
                        P.op("dve", lambda e: e.tensor_reduce(out=Zs[:], in_=e16[:], axis=AX.X, op=ALU.add), reads=["e16"], writes=["Zs"])
                        P.op("act", lambda e: e.activation(out=Zs[:], in_=Zs[:], func=AF.Ln), reads=["Zs"], writes=["Zs"])
                        P.op("dve", lambda e: e.tensor_tensor(out=tmm[:], in0=ts[:, :, 15], in1=ts[:, :, 0], op=ALU.subtract), reads=tkeys, writes=["tmm"])
                        P.op("dve", lambda e, tl=tl: e.tensor_tensor(out=lnw[:, tl, :], in0=tmm[:], in1=Zs[:], op=ALU.subtract), reads=["tmm", "Zs"], writes=[("lnw", tl)])
                        P.op("dve", lambda e, tl=tl: e.tensor_tensor(out=s1pp[:, tl], in0=s1r[:], in1=ts[:, :, 15:16].to_broadcast([128, 8, 128]), op=ALU.subtract),
                             reads=[("s1r", 0), ("s1r", 1)] + tkeys, writes=[("s1pp", tl)])
                    P.flush()
                with contextlib.ExitStack() as st:
                    Tpp = sbt(st, "Tpp", [128, 8, 512], F32)
                    Ew = sbt(st, "Ew", [128, 8, 512], BF16)
                    MEw = sbt(st, "MEw", [128, 8, 512], BF16)
                    gl = sbt(st, "gl", [128, 512], F32)
                    Hg = sbt(st, "Hg", [128, 512], BF16)
                    HgT = sbt(st, "HgT", [128, 512], BF16)
                    x1r = sbt(st, "x1r", [128, D], F32)
                    ot = sbt(st, "ot", [128, D], F32)
                    pA = pst(st, "pA", [128, 512], F32)
                    pB = pst(st, "pB", [128, 512], F32)
                    pC = pst(st, "pC", [128, 1024], BF16)
                    pD = pst(st, "pD", [128, D], F32)
                    for g in range(32):
                        P.dma("sp", lambda e, g=g: e.dma_start(out=UTg[:], in_=utsr[:, :, g * 512:(g + 1) * 512]), "UTg",
                              reads=[("uts", et) for et in range(g * 4, g * 4 + 4)], writes=["UTg"])
                        P.dma("pool", lambda e, g=g: e.dma_start(out=Vg[:], in_=pv[g * 512:(g + 1) * 512, :].rearrange("(i j) d -> j i d", j=128)), "Vg", writes=["Vg"])
                        for tl in range(4):
                            for kc in range(16):
                                P.op("pe", lambda e, kc=kc, tl=tl: e.matmul(pA[:], lhsT=uq[:, kc, tl * 128:(tl + 1) * 128], rhs=UTg[:, kc, :], start=(kc == 0), stop=(kc == 15)),
                                     reads=["uq", "UTg"], writes=["pA"])
                            P.op("pool", lambda e, tl=tl, g=g: e.tensor_tensor(out=Tpp[:].rearrange("p h (i j) -> p h i j", i=4),
                                                                                in0=s1pp[:, tl, :, g * 4:(g + 1) * 4, None].to_broadcast([128, 8, 4, 128]),
                                                                                in1=s2s[:, tl, :, None, :].to_broadcast([128, 8, 4, 128]), op=ALU.add),
                                 reads=[("s1pp", tl), ("s2s", tl, 0), ("s2s", tl, 1)], writes=["Tpp"])
                            for h in range(8):
                                P.op("act", lambda e, h=h, tl=tl: e.activation(out=Ew[:, h, :], in_=Tpp[:, h, :], func=AF.Exp, bias=lnw[:, tl, h:h + 1]),
                                     reads=["Tpp", ("lnw", tl)], writes=[("Ew", h)])
                            P.op("dve", lambda e: e.scalar_tensor_tensor(out=MEw[:], in0=Tpp[:], scalar=-1e-5, in1=Ew[:], op0=ALU.is_ge, op1=ALU.mult),
                                 reads=["Tpp"] + [("Ew", h) for h in range(8)], writes=["MEw"])
                            for h in range(8):
                                P.op("pe", lambda e, h=h: e.matmul(pB[:], lhsT=ident[:], rhs=MEw[:, h, :], start=(h == 0), stop=(h == 7)),
                                     reads=["ident", "MEw"], writes=["pB"])
                            P.op("act", lambda e: e.activation(out=gl[:], in_=pA[:], func=AF.Gelu), reads=["pA"], writes=["gl"])
                            P.op("dve", lambda e: e.tensor_tensor(out=Hg[:], in0=pB[:], in1=gl[:], op=ALU.mult), reads=["pB", "gl"], writes=["Hg"])
                            for i in range(4):
                                P.op("pe", lambda e, i=i: e.transpose(out=pC[:, i * 128:(i + 1) * 128], in_=Hg[:, i * 128:(i + 1) * 128], identity=ident[:]),
                                     reads=["Hg", "ident"], writes=["pC"])
                            P.op("act", lambda e: e.copy(out=HgT[:], in_=pC[:, 0:512]), reads=["pC"], writes=["HgT"])
                            for db in range(4):
                                for i in range(4):
                                    P.op("pe", lambda e, db=db, i=i: e.matmul(pD[:, db * 512:(db + 1) * 512], lhsT=HgT[:, i * 128:(i + 1) * 128], rhs=Vg[:, i, db * 512:(db + 1) * 512],
                                                                              start=(i == 0), stop=(i == 3)),
                                         reads=["HgT", "Vg"], writes=["pD"])
                            if g == 0:
                                P.op("dve", lambda e, tl=tl: e.tensor_copy(out=acc[:, tl, :], in_=pD[:]), reads=["pD"], writes=[("acc", tl)])
                            else:
                                P.op("dve", lambda e, tl=tl: e.tensor_tensor(out=acc[:, tl, :], in0=pD[:], in1=acc[:, tl, :], op=ALU.add),
                                     reads=["pD", ("acc", tl)], writes=[("acc", tl)])
                    for tl in range(4):
                        tt = Q * 4 + tl
                        P.dma("sp", lambda e, tt=tt: e.dma_start(out=x1r[:], in_=y[tt * 128:(tt + 1) * 128, :]), "x1r", reads=[("y", tt)], writes=["x1r"])
                        P.op("dve", lambda e, tl=tl: e.tensor_tensor(out=ot[:], in0=acc[:, tl, :], in1=g2bc[:], op=ALU.mult),
                             reads=[("acc", tl)] + [("g2bc", nb) for nb in range(4)], writes=["ot"])
                        P.op("pool", lambda e: e.tensor_tensor(out=ot[:], in0=ot[:], in1=x1r[:], op=ALU.add), reads=["ot", "x1r"], writes=["ot"])
                        P.dma("sp", lambda e, tt=tt: e.dma_start(out=y[tt * 128:(tt + 1) * 128, :], in_=ot[:]), "ystore2", reads=["ot"], writes=[("y", tt)])
                    P.flush()
    return


RW = 3072

def make_masks():
    idx = np.arange(128)
    same = (idx[:, None] // 64) == (idx[None, :] // 64)
    up = idx[:, None] < idx[None, :]
    lo = idx[:, None] > idx[None, :]
    eq = idx[:, None] == idx[None, :]
    m = np.zeros((128, 5, 256), np.float32)
    m[:, 0, :128] = up & same; m[:, 0, 128:] = lo & same
    m[:, 1, :128] = lo & same; m[:, 1, 128:] = up & same
    m[:, 2, :128] = (up | eq) & same; m[:, 2, 128:] = (up | eq) & same
    m[:, 3, :128] = (lo | eq) & same; m[:, 3, 128:] = (lo | eq) & same
    m[:, 4, :128] = same
    m[:, 4, 128:] = (idx % 64 != 0)[None, :]
    return m

def prep_A(inp):
    w_in = inp["w_in"][0]; mu = inp["rwkv_mu"][0]
    x2 = np.ascontiguousarray(inp["x"].reshape(16384, 2048))
    masks = make_masks()
    maps = []
    for c in range(8):
        ch = slice(128 * c, 128 * c + 128)
        w_rw = np.concatenate([w_in[:, RW + 128 * c: RW + 128 * c + 128], w_in[:, RW + 1024 + 128 * c: RW + 1024 + 128 * c + 128],
                               w_in[:, RW + 2048 + 128 * c: RW + 2048 + 128 * c + 128], w_in[:, RW + 3072: RW + 3712]], axis=1)
        muF = np.zeros((128, 9, 2), np.float32)
        for gi in range(9):
            if gi < 3:
                cols = np.arange(128) + gi * 1024 + 128 * c
            elif gi < 7:
                cols = np.arange(96) + 3072 + 96 * (gi - 3)
            else:
                cols = np.arange(128) + 3456 + 128 * (gi - 7)
            muF[:len(cols), gi, :] = mu[:, cols].T
        chp = np.zeros((128, 12), np.float32)
        chp[:, 0] = inp["rwkv_w0"][0][0, ch]; chp[:, 1] = inp["rwkv_w0"][0][1, ch]
        chp[:, 2] = inp["rwkv_a0"][0][0, ch]; chp[:, 3] = inp["rwkv_a0"][0][1, ch]
        chp[:, 4] = inp["rwkv_k_k"][0][ch]; chp[:, 5] = inp["rwkv_k_a"][0][ch]
        chp[:, 6] = inp["rwkv_r_k"][0].reshape(-1)[ch]; chp[:, 7] = inp["rwkv_ln_w"][0][ch]; chp[:, 8] = inp["rwkv_ln_b"][0][ch]
        maps.append({
            "x": x2, "c2": inp["c"], "ada_w": inp["ada_w"][0], "ada_b": inp["ada_b"][0], "norm1_w": inp["norm1_w"][0],
            "w_rw": np.ascontiguousarray(w_rw), "muF": muF, "chp": chp,
            "w2o": np.ascontiguousarray(inp["rwkv_w2"][0][:, :, ch]), "a2o": np.ascontiguousarray(inp["rwkv_a2"][0][:, :, ch]),
            "g2o": np.ascontiguousarray(inp["rwkv_g2"][0][:, ch]), "masks": masks,
        })
    return maps

GW = 64
NEG = -30000.0

def rowmap(p):
    if p == 0:
        return [4, 5, 6, 7] + list(range(0, 36))
    if p == 3:
        return list(range(92, 128)) + [120, 121, 122, 123]
    return list(range(32 * p - 4, 32 * p + 36))

def bias_table(rpb, p):
    rm = np.array(rowmap(p))
    slots_ip = [5, 0, 1, 14, 15]
    kl = np.arange(128)
    q = np.arange(64)
    out = np.empty((8, 128, 5, 2, 4, 64), np.float32)
    col_start = np.clip(q - 8, 0, 48)
    for s, ip in enumerate(slots_ip):
        for r in range(2):
            il = 2 * ip + r
            gi = 32 * p + il
            rs = min(max(gi - 4, 0), 120)
            for j in range(4):
                br = il + 2 * j + kl // 64
                kc = kl % 64
                gk = rm[br]
                assert np.all((gk >= rs) & (gk < rs + 8)), (p, il, j, gk, rs)
                dr = gk - gi + 7
                dc = np.clip(kc[:, None] - q[None, :], -15, 15) + 15
                mask = (kc[:, None] >= col_start[None, :]) & (kc[:, None] < col_start[None, :] + 16)
                vals = rpb[:, dr[:, None], dc]
                out[:, :, s, r, j, :] = np.where(mask[None], vals, np.float32(NEG))
    return out.reshape(8, 128, 5, 512)

def prep_B(inp, obT_full=None):
    x = inp["x"]
    maps = []
    for c in range(8):
        b, p = c // 4, c % 4
        rm = rowmap(p)
        xg = x[b].reshape(128, GW, 2048)
        xh = np.ascontiguousarray(xg[rm].reshape(40 * GW, 2048))
        m = {
            "xh": xh, "cb": np.ascontiguousarray(inp["c"][b]),
            "ada_w": inp["ada_w"][0], "ada_b": inp["ada_b"][0], "norm1_w": inp["norm1_w"][0], "norm2_w": inp["norm2_w"][0],
            "w_in": inp["w_in"][0], "b_gate": inp["b_gate"][0], "qnw": inp["na_q_norm_w"][0], "knw": inp["na_k_norm_w"][0],
            "biasT": bias_table(inp["na_rpb"][0], p),
            "w_branch_a": inp["w_branch_a"][0], "w_branch_b": inp["w_branch_b"][0], "w_out": inp["w_out"][0],
            "peer_w_q": inp["peer_w_q"][0], "peer_sub_k1": inp["peer_sub_k1"][0], "peer_sub_k2": inp["peer_sub_k2"][0],
            "peer_u": inp["peer_u"][0], "peer_v": inp["peer_v"][0],
        }
        if obT_full is not None:
            m["obT"] = np.ascontiguousarray(obT_full[b][:, p * 2048:(p + 1) * 2048])
        maps.append(m)
    return maps


def kernel(**inputs):
    from concourse.bass_utils import run_bass_kernel_spmd
    inp = {k: np.asarray(v) for k, v in inputs.items()}
    maps_a = prep_A(inp)
    nca = bass.Bass("TRN2", target_bir_lowering=False)
    build_A(nca, stage="all", dbg=False, nblk=32)
    res_a = run_bass_kernel_spmd(nca, maps_a, core_ids=list(range(8)))
    obT_full = np.empty((2, 1024, 8192), np.float32)
    for c in range(8):
        o = res_a.results[c]["obT_out"]
        obT_full[:, 128 * c:128 * (c + 1), :] = o.reshape(128, 2, 8192).transpose(1, 0, 2)
    maps_b = prep_B(inp, obT_full)
    ncb = bass.Bass("TRN2", target_bir_lowering=False)
    build_B(ncb, stages="all", dbg=False)
    names = set()
    for a in ncb.allocations:
        try:
            if a.kind == "ExternalInput":
                names.add(a.memorylocations[0].name)
        except Exception:
            pass
    maps_b = [{k: v for k, v in m.items() if k in names} for m in maps_b]
    res_b = run_bass_kernel_spmd(ncb, maps_b, core_ids=list(range(8)))
    out = np.empty((2, 8192, 2048), np.float32)
    for c in range(8):
        b, p = c // 4, c % 4
        out[b, p * 2048:(p + 1) * 2048] = res_b.results[c]["y"]
    return out
```
